# Optimizing a Trainium2 kernel written in Bass

```python
import jax, jax.numpy as jnp
from jax import lax
import numpy as np

D_MODEL = 4096
BATCH = 4
SEQ = 2048
DEPTH = 1

CHUNK = 64
LEFT_CHUNKS = 8
BAND = (LEFT_CHUNKS + 1) * CHUNK
ATTN_HEAD_DIM = 128
ATTN_HEADS = (D_MODEL // 2) // ATTN_HEAD_DIM
ATTN_WIDTH = ATTN_HEADS * ATTN_HEAD_DIM
MAX_REL_DIST = 256
RET_V_DIM = 256
RET_HEADS = (D_MODEL // 2) // RET_V_DIM
RET_QK_DIM = RET_V_DIM // 2
RET_QK_WIDTH = RET_HEADS * RET_QK_DIM
RET_WIDTH = RET_HEADS * RET_V_DIM
ROPE_BASE = 10000.0
MIX_WIDTH = ATTN_WIDTH + RET_WIDTH
IN_SIZES = (ATTN_WIDTH, ATTN_WIDTH, ATTN_WIDTH, RET_QK_WIDTH, RET_QK_WIDTH, RET_WIDTH, RET_WIDTH)
IN_WIDTH = 3 * ATTN_WIDTH + 2 * RET_QK_WIDTH + 2 * RET_WIDTH
MEM_LEN = 256
XATTN_HEADS = 4
XATTN_HEAD_DIM = D_MODEL // XATTN_HEADS
PEER_HEADS = 8
PEER_KEYS = 128
PEER_EXPERTS = PEER_KEYS * PEER_KEYS
PEER_QUERY_DIM = 256
PEER_HALF = PEER_QUERY_DIM // 2
PEER_TOPK = 16
PEER_TOKEN_BLOCK = 64

EPS = 1e-6
NEG_INF = -1e30

kernel_name = "hybrid_chunkattn_retention_peer_block"


def rms_norm(x, w):
    xf = x.astype(jnp.float32)
    y = xf * lax.rsqrt(jnp.mean(xf * xf, axis=-1, keepdims=True) + EPS)
    return (y * w.astype(jnp.float32)).astype(x.dtype)


def split_columns(t, sizes):
    parts, start = [], 0
    for size in sizes:
        parts.append(t[..., start:start + size])
        start += size
    return parts


def rotary(x, pos):
    d = x.shape[-1]
    inv_freq = 1.0 / (ROPE_BASE ** (jnp.arange(0, d, 2, dtype=jnp.float32) / d))
    ang = pos.astype(jnp.float32)[:, None] * inv_freq[None, :]
    cos = jnp.cos(ang)[None, :, None, :]
    sin = jnp.sin(ang)[None, :, None, :]
    xf = x.astype(jnp.float32)
    x1, x2 = xf[..., : d // 2], xf[..., d // 2:]
    return jnp.concatenate([x1 * cos - x2 * sin, x1 * sin + x2 * cos], axis=-1).astype(x.dtype)


def chunked_rel_attention(q, k, v, rel_bias):
    B, S, H, dh = q.shape
    nc = S // CHUNK

    def to_chunks(t):
        return t.reshape(B, nc, CHUNK, H, dh).transpose(0, 3, 1, 2, 4)

    pad = ((0, 0), (0, 0), (LEFT_CHUNKS, 0), (0, 0), (0, 0))
    qc = to_chunks(q) * (dh ** -0.5)
    kp = jnp.pad(to_chunks(k), pad)
    vp = jnp.pad(to_chunks(v), pad)
    qi = jnp.arange(CHUNK)[:, None]
    band = jnp.arange(BAND)[None, :]
    dist = LEFT_CHUNKS * CHUNK + qi - band
    bias = rel_bias[:, jnp.clip(dist, -MAX_REL_DIST, MAX_REL_DIST) + MAX_REL_DIST]
    src_chunk = jnp.arange(nc)[:, None] - LEFT_CHUNKS + jnp.arange(LEFT_CHUNKS + 1)[None, :]
    valid = jnp.repeat(src_chunk >= 0, CHUNK, axis=1)
    scores = jnp.concatenate(
        [jnp.einsum('bhncd,bhnmd->bhncm', qc, kp[:, :, j:j + nc]) for j in range(LEFT_CHUNKS + 1)],
        axis=-1).astype(jnp.float32)
    scores = scores + bias[None, :, None].astype(jnp.float32)
    scores = jnp.where(valid[None, None, :, None, :], scores, NEG_INF)
    probs = jax.nn.softmax(scores, axis=-1).astype(v.dtype)
    out = jnp.einsum('bhncm,bhnmd->bhncd', probs[..., :CHUNK], vp[:, :, 0:nc])
    for j in range(1, LEFT_CHUNKS + 1):
        out = out + jnp.einsum('bhncm,bhnmd->bhncd',
                               probs[..., j * CHUNK:(j + 1) * CHUNK], vp[:, :, j:j + nc])
    return out.transpose(0, 2, 3, 1, 4).reshape(B, S, H * dh)


def chunkwise_retention(q, k, v, gate, gn_w):
    B, S, H, dk = q.shape
    dv = v.shape[-1]
    nc = S // CHUNK
    dt = q.dtype
    log_gamma = jnp.log1p(-jnp.power(2.0, -5.0 - jnp.arange(H, dtype=jnp.float32)))
    pos = jnp.arange(CHUNK, dtype=jnp.float32)
    inner_decay = jnp.exp(log_gamma[:, None, None] * jnp.abs(pos[:, None] - pos[None, :]))
    q_decay = jnp.exp(log_gamma[:, None] * (pos + 1.0))
    k_decay = jnp.exp(log_gamma[:, None] * (CHUNK - 1.0 - pos))
    chunk_decay = jnp.exp(log_gamma * CHUNK)[None, :, None, None]

    qc = q.reshape(B, nc, CHUNK, H, dk).transpose(0, 3, 1, 2, 4)
    kc = k.reshape(B, nc, CHUNK, H, dk).transpose(0, 3, 1, 2, 4) * (dk ** -0.5)
    vc = v.reshape(B, nc, CHUNK, H, dv).transpose(0, 3, 1, 2, 4)

    inner = jnp.einsum('bhncd,bhnmd->bhncm', qc, kc) * inner_decay[None, :, None].astype(dt)
    inner = jnp.einsum('bhncm,bhnme->bhnce', inner, vc)
    kv = jnp.einsum('bhncd,bhnce->nbhde',
                    kc * k_decay[None, :, None, :, None].astype(dt), vc)

    def step(state, kv_n):
        return state * chunk_decay + kv_n, state

    _, states = lax.scan(step, jnp.zeros((B, H, dk, dv), jnp.float32), kv.astype(jnp.float32))
    cross = jnp.einsum('bhncd,nbhde->bhnce',
                       qc * q_decay[None, :, None, :, None].astype(dt), states.astype(dt))
    y = (inner + cross).transpose(0, 2, 3, 1, 4).astype(jnp.float32)
    mu = jnp.mean(y, axis=-1, keepdims=True)
    var = jnp.mean(jnp.square(y - mu), axis=-1, keepdims=True)
    yn = ((y - mu) * lax.rsqrt(var + EPS)).reshape(B, S, H * dv) * gn_w.astype(jnp.float32)
    return (jax.nn.silu(gate.astype(jnp.float32)) * yn).astype(dt)


def memory_cross_attention(h, mem_n, wq, wkv, wo):
    B, S, D = h.shape
    M = mem_n.shape[1]
    q = (h @ wq).reshape(B, S, XATTN_HEADS, XATTN_HEAD_DIM)
    k, v = split_columns(mem_n @ wkv, (D, D))
    k = k.reshape(B, M, XATTN_HEADS, XATTN_HEAD_DIM)
    v = v.reshape(B, M, XATTN_HEADS, XATTN_HEAD_DIM)
    s = jnp.einsum('bshd,bmhd->bhsm', q, k).astype(jnp.float32) * (XATTN_HEAD_DIM ** -0.5)
    p = jax.nn.softmax(s, axis=-1).astype(v.dtype)
    o = jnp.einsum('bhsm,bmhd->bshd', p, v).reshape(B, S, D)
    return o @ wo


def peer_ffn(h, wq, subkeys, u, v):
    B, S, D = h.shape
    T = B * S
    x = h.reshape(T, D)
    q = (x @ wq).reshape(T, PEER_HEADS, 2, PEER_HALF)
    s = jnp.einsum('thpd,hpkd->thpk', q, subkeys).astype(jnp.float32)
    top_s, top_i = lax.top_k(s, PEER_TOPK)
    cand_s = top_s[:, :, 0, :, None] + top_s[:, :, 1, None, :]
    cand_i = top_i[:, :, 0, :, None] * PEER_KEYS + top_i[:, :, 1, None, :]
    best_s, best_c = lax.top_k(cand_s.reshape(T, PEER_HEADS, PEER_TOPK * PEER_TOPK), PEER_TOPK)
    expert_idx = jnp.take_along_axis(
        cand_i.reshape(T, PEER_HEADS, PEER_TOPK * PEER_TOPK), best_c, axis=-1)
    gates = jax.nn.softmax(best_s, axis=-1).astype(h.dtype)
    nb = T // PEER_TOKEN_BLOCK
    xb = x.reshape(nb, PEER_TOKEN_BLOCK, D)
    ib = expert_idx.reshape(nb, PEER_TOKEN_BLOCK, PEER_HEADS * PEER_TOPK)
    gb = gates.reshape(nb, PEER_TOKEN_BLOCK, PEER_HEADS * PEER_TOPK)

    def block(args):
        xt, idx, g = args
        act = jax.nn.gelu(jnp.einsum('td,tkd->tk', xt, u[idx]), approximate=False)
        return jnp.einsum('tk,tkd->td', g * act, v[idx])

    out = lax.map(block, (xb, ib, gb))
    return out.reshape(B, S, D)


def setup_inputs(seed: int = 0) -> dict:
    key = jax.random.key(seed)
    ks = jax.random.split(key, 18)
    f32 = jnp.float32
    L = DEPTH

    def nrm(k, shape, scale):
        return jax.random.normal(k, shape, f32) * scale

    def gain(k, shape):
        return 1.0 + 0.01 * jax.random.normal(k, shape, f32)

    return {
        "x": nrm(ks[0], (BATCH, SEQ, D_MODEL), 1.0),
        "mem": nrm(ks[1], (BATCH, MEM_LEN, D_MODEL), 1.0),
        "norm1_w": gain(ks[2], (L, D_MODEL)),
        "w_in": nrm(ks[3], (L, D_MODEL, IN_WIDTH), D_MODEL ** -0.5),
        "attn_rel_bias": nrm(ks[4], (L, ATTN_HEADS, 2 * MAX_REL_DIST + 1), 0.1),
        "ret_gn_w": gain(ks[5], (L, RET_WIDTH)),
        "w_out": nrm(ks[6], (L, MIX_WIDTH, D_MODEL), MIX_WIDTH ** -0.5),
        "norm2_w": gain(ks[7], (L, D_MODEL)),
        "mem_norm_w": gain(ks[8], (L, D_MODEL)),
        "xattn_wq": nrm(ks[9], (L, D_MODEL, D_MODEL), D_MODEL ** -0.5),
        "xattn_wkv": nrm(ks[10], (L, D_MODEL, 2 * D_MODEL), D_MODEL ** -0.5),
        "xattn_wo": nrm(ks[11], (L, D_MODEL, D_MODEL), D_MODEL ** -0.5),
        "norm3_w": gain(ks[12], (L, D_MODEL)),
        "peer_wq": nrm(ks[13], (L, D_MODEL, PEER_HEADS * PEER_QUERY_DIM), D_MODEL ** -0.5),
        "peer_subkeys": nrm(ks[14], (L, PEER_HEADS, 2, PEER_KEYS, PEER_HALF), PEER_HALF ** -0.5),
        "peer_u": nrm(ks[15], (L, PEER_EXPERTS, D_MODEL), D_MODEL ** -0.5),
        "peer_v": nrm(ks[16], (L, PEER_EXPERTS, D_MODEL), PEER_HEADS ** -0.5),
        "final_norm_w": gain(ks[17], (D_MODEL,)),
    }


def reference(x, mem, norm1_w, w_in, attn_rel_bias, ret_gn_w, w_out, norm2_w, mem_norm_w,
              xattn_wq, xattn_wkv, xattn_wo, norm3_w, peer_wq, peer_subkeys, peer_u, peer_v,
              final_norm_w):
    B, S, _ = x.shape
    pos = jnp.arange(S)
    h = x
    for l in range(DEPTH):
        xn = rms_norm(h, norm1_w[l])
        aq, ak, av, rq, rk, rv, rg = split_columns(xn @ w_in[l], IN_SIZES)
        a_out = chunked_rel_attention(
            aq.reshape(B, S, ATTN_HEADS, ATTN_HEAD_DIM),
            ak.reshape(B, S, ATTN_HEADS, ATTN_HEAD_DIM),
            av.reshape(B, S, ATTN_HEADS, ATTN_HEAD_DIM),
            attn_rel_bias[l])
        r_out = chunkwise_retention(
            rotary(rq.reshape(B, S, RET_HEADS, RET_QK_DIM), pos),
            rotary(rk.reshape(B, S, RET_HEADS, RET_QK_DIM), pos),
            rv.reshape(B, S, RET_HEADS, RET_V_DIM),
            rg, ret_gn_w[l])
        h = h + jnp.concatenate([a_out, r_out], axis=-1) @ w_out[l]
        mem_n = rms_norm(mem, mem_norm_w[l])
        h = h + memory_cross_attention(rms_norm(h, norm2_w[l]), mem_n,
                                       xattn_wq[l], xattn_wkv[l], xattn_wo[l])
        h = h + peer_ffn(rms_norm(h, norm3_w[l]), peer_wq[l], peer_subkeys[l], peer_u[l], peer_v[l])
    return rms_norm(h, final_norm_w)
```

```python
import contextlib
import math
import numpy as np
import concourse.bass as bass
import concourse.mybir as mybir
from concourse.bass_utils import run_bass_kernel_spmd

F32 = mybir.dt.float32
BF16 = mybir.dt.bfloat16
U32 = mybir.dt.uint32
AF = mybir.ActivationFunctionType
ALU = mybir.AluOpType
AX = mybir.AxisListType

EPS = 1e-6
NEG = -1e30
ENGS = ("tensor", "vector", "scalar", "gpsimd", "sync")


class Op:
    __slots__ = ("eng", "fn", "waits", "signal", "seq", "is_dma", "dsem", "dval")

    def __init__(self, eng, fn, is_dma):
        self.eng = eng
        self.fn = fn
        self.is_dma = is_dma
        self.waits = []
        self.signal = False
        self.seq = None
        self.dsem = None
        self.dval = None


class Prog:
    def __init__(self, nc, n_dma_sems=24):
        self.nc = nc
        self.ops = {e: [] for e in ENGS}
        self.last_w = {}
        self.readers = {}
        self.n_dma_sems = n_dma_sems
        self.dma_rr = {e: 0 for e in ENGS}
        self.dma_last = {}
        self.dma_cnt = {}
        self.pending_barrier = {e: None for e in ENGS}

    def barrier(self):
        b = []
        for e in ENGS:
            for op in reversed(self.ops[e]):
                if not op.is_dma:
                    b.append(op)
                    break
        b.extend(self.dma_last.values())
        for e in ENGS:
            self.pending_barrier[e] = b
        self.last_w = {}
        self.readers = {}

    def add(self, eng, fn, reads=(), writes=(), dma=False):
        op = Op(eng, fn, dma)
        w = op.waits
        pb = self.pending_barrier[eng]
        if pb is not None:
            w.extend(p for p in pb if p is not op)
            self.pending_barrier[eng] = None
        for k in reads:
            p = self.last_w.get(k)
            if p is not None:
                w.append(p)
        for k in writes:
            p = self.last_w.get(k)
            if p is not None:
                w.append(p)
            r = self.readers.get(k)
            if r:
                w.extend(r)
        for k in reads:
            self.readers.setdefault(k, []).append(op)
        for k in writes:
            self.last_w[k] = op
            self.readers[k] = []
        if dma:
            slot = self.dma_rr[eng]
            self.dma_rr[eng] = (slot + 1) % self.n_dma_sems
            prev = self.dma_last.get((eng, slot))
            if prev is not None:
                w.append(prev)
            self.dma_last[(eng, slot)] = op
            c = self.dma_cnt.get((eng, slot), 0) + 1
            self.dma_cnt[(eng, slot)] = c
            op.dsem = (eng, slot)
            op.dval = 16 * c
        self.ops[eng].append(op)
        return op

    def emit(self):
        nc = self.nc
        for e in ENGS:
            for op in self.ops[e]:
                for p in op.waits:
                    if not p.is_dma:
                        p.signal = True
        for e in ENGS:
            n = 0
            for op in self.ops[e]:
                if op.signal and not op.is_dma:
                    n += 1
                    op.seq = n
        with contextlib.ExitStack() as st:
            esem = {e: st.enter_context(nc.semaphore("es_" + e)) for e in ENGS}
            dsem = {}
            for e in ENGS:
                if any(o.is_dma for o in self.ops[e]):
                    for s in range(self.n_dma_sems):
                        dsem[(e, s)] = st.enter_context(nc.semaphore("ds_%s_%d" % (e, s)))
            block = st.enter_context(nc.Block())

            def run(eng_name):
                def body(eng):
                    known_e = {}
                    known_d = {}
                    for op in self.ops[eng_name]:
                        need_e = {}
                        need_d = {}
                        for p in op.waits:
                            if p.is_dma:
                                if need_d.get(p.dsem, 0) < p.dval:
                                    need_d[p.dsem] = p.dval
                            else:
                                if need_e.get(p.eng, 0) < p.seq:
                                    need_e[p.eng] = p.seq
                        for k, v in need_e.items():
                            if known_e.get(k, 0) < v:
                                eng.wait_ge(esem[k], v)
                                known_e[k] = v
                        for k, v in need_d.items():
                            if known_d.get(k, 0) < v:
                                eng.wait_ge(dsem[k], v)
                                known_d[k] = v
                        ins = op.fn(eng)
                        if op.is_dma:
                            ins.then_inc(dsem[op.dsem], 16)
                        elif op.signal:
                            ins.then_inc(esem[eng_name], 1)
                    if eng_name == "sync":
                        for (e, s), c in self.dma_cnt.items():
                            eng.wait_ge(dsem[(e, s)], 16 * c)
                return body

            for e in ENGS:
                if self.ops[e] or e == "sync":
                    getattr(block, e)(run(e))


class Tl:
    __slots__ = ("ap", "key")

    def __init__(self, ap, key):
        self.ap = ap
        self.key = key

    def __getitem__(self, idx):
        return self.ap[idx]


_DT_BYTES = {F32: 4, BF16: 2, U32: 4}


class Arena:
    def __init__(self, nc, st, nbytes):
        self.t = st.enter_context(nc.sbuf_tensor("arena", [128, nbytes // 4], F32))
        self.words = nbytes // 4
        self.off = 0
        self.gen = 0

    def reset(self):
        self.off = 0
        self.gen += 1

    def alloc(self, name, free_shape, dt):
        n = 1
        for s in free_shape:
            n *= s
        words = (n * _DT_BYTES[dt] + 3) // 4
        words = (words + 7) // 8 * 8
        assert self.off + words <= self.words, "arena overflow at %s: %d + %d > %d" % (name, self.off * 4, words * 4, self.words * 4)
        ap = self.t[:, self.off:self.off + words]
        self.off += words
        if dt != F32:
            ap = ap.bitcast(dt)
        ap = ap[:, 0:n]
        if len(free_shape) == 2:
            ap = ap.rearrange("p (a b) -> p a b", a=free_shape[0])
        elif len(free_shape) == 3:
            ap = ap.rearrange("p (a b c) -> p a b c", a=free_shape[0], b=free_shape[1])
        elif len(free_shape) == 4:
            ap = ap.rearrange("p (a b c d) -> p a b c d", a=free_shape[0], b=free_shape[1], c=free_shape[2])
        return Tl(ap, "%s#%d" % (name, self.gen))


def build_program(D, T, nph=None, dbg=False, stub_peer=False):
    KC = D // 128
    W2 = 2 * T
    NTB = T // 512
    NWB = W2 // 512
    NTT = T // 128
    NWT = W2 // 128
    HA = D // 256
    HR = D // 512
    NAB = 1 + NTB
    NKP = NAB * 4
    XH = 4
    DX = D // XH
    CX = DX // 128
    MEM = 256
    NE = 128
    gam = [1.0 - 2.0 ** (-5.0 - h) for h in range(HR)]

    nc = bass.Bass("TRN2", target_bir_lowering=False)

    def din(name, shape, dt=F32):
        return nc.dram_tensor(name, list(shape), dt, kind="ExternalInput").ap()

    def dscr(name, shape, dt):
        if dbg:
            return nc.dram_tensor(name, list(shape), dt, kind="ExternalOutput").ap()
        return nc.dram_tensor(name, list(shape), dt).ap()

    xw = din("xw", [W2, D])
    memb = din("memb", [MEM, D])
    n1w = din("n1w", [128, D])
    n2w = din("n2w", [128, D])
    nmw = din("nmw", [128, D])
    n3w = din("n3w", [128, D])
    nfw = din("nfw", [128, D])
    w_attn = din("w_attn", [D, HA * 384])
    w_ret = din("w_ret", [D, HR * 768])
    relbT = din("relbT", [HA, 128, 640])
    kmask = din("kmask", [128, NKP])
    costab = din("costab", [128, W2])
    sintab = din("sintab", [128, W2])
    mtab = din("mtab", [128, HR * 128])
    gtab = din("gtab", [128, HR * 128])
    kdec = din("kdec", [128, HR * NWT])
    gnw = din("gnw", [128, HR * 256])
    w_out = din("w_out", [D, D])
    wq = din("wq", [D, D])
    wkv = din("wkv", [D, 2 * D])
    wo = din("wo", [D, D])
    pwq = din("pwq", [D, 2048])
    subT = din("subT", [128, 16 * 128])
    uT = din("uT", [D, NE * 128] if not stub_peer else [128, 128])
    pv = din("pv", [NE * 128, D] if not stub_peer else [128, 128])
    consts = din("consts", [128, 4 * 128])
    out = nc.dram_tensor("out", [T, D], F32, kind="ExternalOutput").ap()

    xnT_d = dscr("xnT_d", [NWB, 128, KC, 512], BF16)
    mixT_d = dscr("mixT_d", [NTB, 128, KC, 512], BF16)
    h1_d = dscr("h1_d", [T, D], F32)
    hn2T_d = dscr("hn2T_d", [NTB, 128, KC, 512], BF16)
    memT_d = dscr("memT_d", [1, 128, KC, MEM], BF16)
    oT_d = dscr("oT_d", [NTB, 128, KC, 512], BF16)
    h2_d = dscr("h2_d", [T, D], F32)
    hn3T_d = dscr("hn3T_d", [NTB, 128, KC, 512], BF16)
    G_d = dscr("G_d", [NE, 128, T], BF16)
    GA_d = dscr("GA_d", [NE, 128, T], BF16)
    h3_d = dscr("h3_d", [T, D], F32)

    P = Prog(nc)
    st = contextlib.ExitStack()
    with st:
        A = Arena(nc, st, 200 * 1024)
        pb = [Tl(st.enter_context(nc.psum_tensor("pb%d" % i, [128, 512], F32))[:], "pb%d" % i) for i in range(8)]
        cst_f = st.enter_context(nc.sbuf_tensor("cst_f", [128, 512], F32))
        cst_b = st.enter_context(nc.sbuf_tensor("cst_b", [128, 512], BF16))
        identf = cst_f[:, 0:128]
        iotaf = cst_f[:, 384:512]
        identb = cst_b[:, 0:128]
        permb = cst_b[:, 128:256]
        onesb = cst_b[:, 256:384]
        CK = ["cst"]

        def DMA(eng, o, i, reads, writes):
            P.add(eng, lambda e: e.dma_start(out=o, in_=i), reads, writes, dma=True)

        def MM(o, lhsT, rhs, start, stop, reads, writes):
            P.add("tensor", lambda e: e.matmul(o, lhsT=lhsT, rhs=rhs, start=start, stop=stop), reads, writes)

        def TR(o, i, ident, reads, writes):
            P.add("tensor", lambda e: e.transpose(out=o, in_=i, identity=ident), reads, writes)

        def ACT(o, i, func, reads, writes, bias=None, scale=None, accum=None):
            kw = {}
            if bias is not None:
                kw["bias"] = bias
            if scale is not None:
                kw["scale"] = scale
            if accum is not None:
                kw["accum_out"] = accum
            P.add("scalar", lambda e: e.activation(out=o, in_=i, func=func, **kw), reads, writes)

        def CP(eng, o, i, reads, writes):
            if eng == "scalar":
                P.add("scalar", lambda e: e.activation(out=o, in_=i, func=AF.Copy), reads, writes)
            else:
                P.add(eng, lambda e: e.tensor_copy(out=o, in_=i), reads, writes)

        def TT(o, a, b, op, reads, writes, eng="vector"):
            P.add(eng, lambda e: e.tensor_tensor(out=o, in0=a, in1=b, op=op), reads, writes)

        def TS(o, a, s1, s2, op0, op1, reads, writes, eng="vector"):
            if op1 is None:
                P.add(eng, lambda e: e.tensor_scalar(out=o, in0=a, scalar1=s1, scalar2=None, op0=op0), reads, writes)
            else:
                P.add(eng, lambda e: e.tensor_scalar(out=o, in0=a, scalar1=s1, scalar2=s2, op0=op0, op1=op1), reads, writes)

        def STT(o, a, s, b, op0, op1, reads, writes):
            P.add("vector", lambda e: e.scalar_tensor_tensor(out=o, in0=a, scalar=s, in1=b, op0=op0, op1=op1), reads, writes)

        def RECIP(o, i, reads, writes):
            P.add("vector", lambda e: e.reciprocal(out=o, in_=i), reads, writes)

        def wview(w_ap, c0, c1):
            return w_ap[:, c0:c1].rearrange("(kc p) n -> p kc n", p=128)

        def load_w(dst, w_ap, c0, c1, kcn, reads=()):
            v = wview(w_ap, c0, c1)
            step = max(1, 1024 // 128)
            for k0 in range(0, kcn, step):
                k1 = min(kcn, k0 + step)
                for n0 in range(0, c1 - c0, 512):
                    n1 = min(c1 - c0, n0 + 512)
                    DMA("gpsimd", dst.ap[:, k0:k1, n0:n1], v[:, k0:k1, n0:n1], list(reads), [dst.key])

        evac_rr = [0]

        def evac_eng():
            evac_rr[0] += 1
            return "scalar" if evac_rr[0] % 2 else "vector"

        DMA("sync", cst_f[:], consts, [], CK)
        CP("vector", cst_b[:], cst_f[:], CK, ["cstb"])
        CKB = ["cstb"]

        def phase_nt(src, ntok, wrep_d, dst_d, blk, final_out=None):
            A.reset()
            wrep = A.alloc("wrep", [D], F32)
            xt = [A.alloc("xt%d" % i, [D], F32) for i in range(2)]
            junk = A.alloc("junk", [D], BF16)
            ss = [A.alloc("ss%d" % i, [1], F32) for i in range(2)]
            rs = [A.alloc("rs%d" % i, [1], F32) for i in range(2)]
            if final_out is None:
                xb = [A.alloc("xb%d" % i, [D], BF16) for i in range(2)]
                xT = [A.alloc("xT%d" % i, [KC, 128], BF16) for i in range(2)]
            else:
                xo = [A.alloc("xo%d" % i, [D], F32) for i in range(2)]
            DMA("sync", wrep.ap, wrep_d, [], [wrep.key])
            nt = ntok // 128
            per = blk // 128
            bank = 0
            for i in range(nt):
                s = i % 2
                DMA("sync", xt[s].ap, src[i * 128:(i + 1) * 128, :], [], [xt[s].key])
                ACT(junk.ap, xt[s].ap, AF.Square, [xt[s].key], [junk.key, ss[s].key], accum=ss[s].ap)
                TS(rs[s].ap, ss[s].ap, 1.0 / D, EPS, ALU.mult, ALU.add, [ss[s].key], [rs[s].key])
                ACT(rs[s].ap, rs[s].ap, AF.Sqrt, [rs[s].key], [rs[s].key])
                RECIP(rs[s].ap, rs[s].ap, [rs[s].key], [rs[s].key])
                if final_out is not None:
                    STT(xo[s].ap, xt[s].ap, rs[s].ap, wrep.ap, ALU.mult, ALU.mult, [xt[s].key, rs[s].key, wrep.key], [xo[s].key])
                    DMA("sync", final_out[i * 128:(i + 1) * 128, :], xo[s].ap, [xo[s].key], [])
                    continue
                STT(xb[s].ap, xt[s].ap, rs[s].ap, wrep.ap, ALU.mult, ALU.mult, [xt[s].key, rs[s].key, wrep.key], [xb[s].key])
                for g in range(KC // 8):
                    pt = pb[bank % 4]
                    bank += 1
                    ptv = pt.ap.bitcast(BF16).rearrange("p (a b) -> p a b", a=8)
                    for j in range(8):
                        c = g * 8 + j
                        TR(ptv[:, j, :], xb[s].ap[:, c * 128:(c + 1) * 128], identb, [xb[s].key] + CKB, [pt.key])
                    CP(evac_eng(), xT[s].ap[:, g * 8:(g + 1) * 8, :], ptv, [pt.key], [xT[s].key])
                b_, sub = divmod(i, per)
                DMA("sync", dst_d[b_, :, :, sub * 128:(sub + 1) * 128], xT[s].ap, [xT[s].key], [dst_d.tensor.name])

        def phase_attn():
            A.reset()
            wA = [A.alloc("wA%d" % i, [KC, 384], BF16) for i in range(2)]
            xblk = [A.alloc("xblk%d" % i, [KC, 512], BF16) for i in range(2)]
            km = A.alloc("km", [NKP], F32)
            qT = A.alloc("qT", [T], BF16)
            kT = A.alloc("kT", [NAB * 512], BF16)
            vA = A.alloc("vA", [NKP, 128], BF16)
            bT = [A.alloc("bT%d" % i, [640], BF16) for i in range(2)]
            PT = A.alloc("PT", [NKP, 640], BF16)
            oT = [A.alloc("oT%d" % i, [T], BF16) for i in range(2)]
            rc = [A.alloc("rc%d" % i, [128], F32) for i in range(2)]
            ovs = [A.alloc("ov%d" % i, [128], F32) for i in range(2)]
            DMA("sync", km.ap, kmask, [], [km.key])
            xcnt = 0
            sc = 128 ** -0.5
            XD = [xnT_d.tensor.name]
            for h in range(HA):
                w = wA[h % 2]
                load_w(w, w_attn, h * 384, (h + 1) * 384, KC)
                b = bT[h % 2]
                DMA("gpsimd", b.ap, relbT[h], [], [b.key])
                for tbi in range(NAB):
                    tb = NWB - NAB + tbi
                    xb = xblk[xcnt % 2]
                    xcnt += 1
                    DMA("sync", xb.ap, xnT_d[tb], XD, [xb.key])
                    pk = pb[0]
                    for kc in range(KC):
                        MM(pk.ap, w.ap[:, kc, 128:256], xb.ap[:, kc, :], kc == 0, kc == KC - 1, [w.key, xb.key], [pk.key])
                    CP("scalar", kT.ap[:, tbi * 512:(tbi + 1) * 512], pk.ap, [pk.key], [kT.key])
                    if tbi >= 1:
                        pq = pb[1]
                        for kc in range(KC):
                            MM(pq.ap, w.ap[:, kc, 0:128], xb.ap[:, kc, :], kc == 0, kc == KC - 1, [w.key, xb.key], [pq.key])
                        TS(qT.ap[:, (tbi - 1) * 512:tbi * 512], pq.ap, sc, None, ALU.mult, None, [pq.key], [qT.key])
                    pvv = pb[2 + (tbi % 2)]
                    for j in range(4):
                        for kc in range(KC):
                            MM(pvv.ap[:, j * 128:(j + 1) * 128], xb.ap[:, kc, j * 128:(j + 1) * 128], w.ap[:, kc, 256:384],
                               kc == 0, kc == KC - 1, [w.key, xb.key], [pvv.key])
                    CP("vector", vA.ap[:, tbi * 4:(tbi + 1) * 4, :], pvv.ap.rearrange("p (a b) -> p a b", a=4), [pvv.key], [vA.key])
                import os as _os
                _cut = int(_os.environ.get('ATTN_CUT', '9'))
                if _cut < 2:
                    continue
                for kt in range(NKP):
                    q_lo = max(kt, 4)
                    q_hi = min(kt + 4, NKP - 1)
                    nq = (q_hi - q_lo + 1) * 128
                    jq0 = q_lo - kt
                    parts = [(0, min(nq, 512))]
                    if nq > 512:
                        parts.append((512, nq))
                    for pi, (c0, c1) in enumerate(parts):
                        ps = pb[4 + ((2 * kt + pi) % 4)]
                        n = c1 - c0
                        MM(ps.ap[:, 0:n], kT.ap[:, kt * 128:(kt + 1) * 128], qT.ap[:, (q_lo - 4) * 128 + c0:(q_lo - 4) * 128 + c1],
                           True, False, [kT.key, qT.key], [ps.key])
                        MM(ps.ap[:, 0:n], identb, b.ap[:, jq0 * 128 + c0:jq0 * 128 + c1], False, True, [b.key] + CKB, [ps.key])
                        ACT(PT.ap[:, kt, c0:c1], ps.ap[:, 0:n], AF.Exp, [ps.key, km.key], [PT.key + ":%d" % kt], bias=km.ap[:, kt:kt + 1])
                if _cut < 3:
                    continue
                o = oT[h % 2]
                for qo in range(NTT):
                    qt = qo + 4
                    po = pb[(qo % 2)]
                    kts = list(range(qt - 4, qt + 1))
                    for gi, (lhs, c0) in enumerate(((None, 0), (onesb, 128))):
                        for n_, kt in enumerate(kts):
                            off = (qt - max(kt, 4)) * 128
                            l = vA.ap[:, kt, :] if lhs is None else lhs
                            MM(po.ap[:, c0:c0 + 128], l, PT.ap[:, kt, off:off + 128], n_ == 0, n_ == len(kts) - 1,
                               [vA.key, PT.key + ":%d" % kt] + CKB, [po.key])
                    r = rc[qo % 2]
                    CP("scalar", r.ap, po.ap[:, 128:256], [po.key], [r.key])
                    RECIP(r.ap, r.ap, [r.key], [r.key])
                    ov = ovs[qo % 2]
                    CP("scalar", ov.ap, po.ap[:, 0:128], [po.key], [ov.key])
                    TT(o.ap[:, qo * 128:(qo + 1) * 128], ov.ap, r.ap, ALU.mult, [ov.key, r.key], [o.key])
                for tb in range(NTB):
                    DMA("sync", mixT_d[tb, :, h, :], o.ap[:, tb * 512:(tb + 1) * 512], [o.key], [mixT_d.tensor.name])

        def phase_ret():
            A.reset()
            W = A.alloc("wR", [KC, 768], BF16)
            xblk = [A.alloc("xblk%d" % i, [KC, 512], BF16) for i in range(2)]
            cs = [A.alloc("cs%d" % i, [512], F32) for i in range(2)]
            sn = [A.alloc("sn%d" % i, [512], F32) for i in range(2)]
            mt = A.alloc("mt", [HR, 128], F32)
            gt = A.alloc("gt", [HR, 128], F32)
            kd = A.alloc("kd", [HR, NWT], F32)
            gw = A.alloc("gw", [HR, 256], F32)
            krT = A.alloc("krT", [W2], BF16)
            qrT = A.alloc("qrT", [T], BF16)
            qtT = A.alloc("qtT", [T], BF16)
            ktm = A.alloc("ktm", [NWT, 128], BF16)
            vR = A.alloc("vR", [NWT, 256], BF16)
            sg = A.alloc("sg", [NTT, 256], BF16)
            raw = [A.alloc("raw%d" % i, [512], BF16) for i in range(2)]
            raw2 = [A.alloc("raw2%d" % i, [512], BF16) for i in range(2)]
            t1 = [A.alloc("t1%d" % i, [512], F32) for i in range(2)]
            t2 = [A.alloc("t2%d" % i, [512], F32) for i in range(2)]
            S = A.alloc("S", [256], F32)
            Sb = A.alloc("Sb", [256], BF16)
            AT = [A.alloc("AT%d" % i, [128], BF16) for i in range(2)]
            st6 = A.alloc("st6", [6], F32)
            mv = A.alloc("mv", [2], F32)
            rstd = A.alloc("rstd", [1], F32)
            yn = [A.alloc("yn%d" % i, [256], F32) for i in range(2)]
            rr = [A.alloc("rr%d" % i, [256], BF16) for i in range(2)]
            rT = A.alloc("rT", [2, T], BF16)
            tis = [A.alloc("ti%d" % i, [128], F32) for i in range(2)]
            kvs = A.alloc("kvs", [256], F32)
            ysbs = [A.alloc("ysb%d" % i, [256], F32) for i in range(2)]
            import os as _os
            _sub = _os.environ.get('RET_SUB', '')
            if 'd' not in _sub:
                DMA("sync", mt.ap, mtab.rearrange("p (h c) -> p h c", h=HR), [], [mt.key])
                DMA("sync", gt.ap, gtab.rearrange("p (h c) -> p h c", h=HR), [], [gt.key])
                DMA("sync", kd.ap, kdec.rearrange("p (h c) -> p h c", h=HR), [], [kd.key])
                DMA("sync", gw.ap, gnw.rearrange("p (h c) -> p h c", h=HR), [], [gw.key])
            XD = [xnT_d.tensor.name]
            xcnt = 0
            rcnt = 0

            def rotary(ps, dst_ap, dst_key, c, s_):
                nonlocal rcnt
                i = rcnt % 2
                rcnt += 1
                if 'r' in _sub:
                    CP("scalar", dst_ap, ps.ap, [ps.key], [dst_key])
                    return
                _rn = int(_os.environ.get('ROT_N', '9'))
                CP("scalar", raw[i].ap, ps.ap, [ps.key], [raw[i].key])
                psw = pb[6 + i]
                if _rn >= 2:
                    MM(psw.ap, permb, raw[i].ap, True, True, [raw[i].key] + CKB, [psw.key])
                if _rn >= 3:
                    TT(t1[i].ap, raw[i].ap, c.ap, ALU.mult, [raw[i].key, c.key], [t1[i].key])
                if _rn >= 4:
                    CP("scalar", raw2[i].ap, psw.ap, [psw.key], [raw2[i].key])
                    TT(t2[i].ap, raw2[i].ap, s_.ap, ALU.mult, [raw2[i].key, s_.key], [t2[i].key])
                if _rn >= 5:
                    TT(dst_ap, t1[i].ap, t2[i].ap, ALU.add, [t1[i].key, t2[i].key], [dst_key])

            import os as _os
            _sub = _os.environ.get('RET_SUB', '')
            for h in range(HR if 'x' not in _sub else 0):
                load_w(W, w_ret, h * 768, (h + 1) * 768, KC)
                if 'w' in _sub:
                    continue
                for tb in range(NWB):
                    own = tb >= NWB // 2
                    ob = tb - NWB // 2
                    xb = xblk[xcnt % 2]
                    c_ = cs[xcnt % 2]
                    s_ = sn[xcnt % 2]
                    xcnt += 1
                    DMA("sync", xb.ap, xnT_d[tb], XD, [xb.key])
                    DMA("sync", c_.ap, costab[:, tb * 512:(tb + 1) * 512], [], [c_.key])
                    DMA("sync", s_.ap, sintab[:, tb * 512:(tb + 1) * 512], [], [s_.key])
                    pk = pb[0]
                    for kc in range(KC):
                        MM(pk.ap, W.ap[:, kc, 128:256], xb.ap[:, kc, :], kc == 0, kc == KC - 1, [W.key, xb.key], [pk.key])
                    rotary(pk, krT.ap[:, tb * 512:(tb + 1) * 512], krT.key + ":%d" % tb, c_, s_)
                    if own and 'q' not in _sub:
                        pq = pb[1]
                        for kc in range(KC):
                            MM(pq.ap, W.ap[:, kc, 0:128], xb.ap[:, kc, :], kc == 0, kc == KC - 1, [W.key, xb.key], [pq.key])
                        rotary(pq, qrT.ap[:, ob * 512:(ob + 1) * 512], qrT.key + ":%d" % ob, c_, s_)
                        TT(qtT.ap[:, ob * 512:(ob + 1) * 512].rearrange("p (a b) -> p a b", a=4),
                           qrT.ap[:, ob * 512:(ob + 1) * 512].rearrange("p (a b) -> p a b", a=4),
                           gt.ap[:, h:h + 1, :].broadcast_to([128, 4, 128]), ALU.mult,
                           [qrT.key + ":%d" % ob, gt.key], [qtT.key + ":%d" % ob])
                    for j in range(4 if 'v' not in _sub else 0):
                        wt = tb * 4 + j
                        pvv = pb[2 + (j % 2)]
                        for kc in range(KC):
                            MM(pvv.ap[:, 0:256], xb.ap[:, kc, j * 128:(j + 1) * 128], W.ap[:, kc, 256:512], kc == 0, kc == KC - 1,
                               [W.key, xb.key], [pvv.key])
                        CP(evac_eng(), vR.ap[:, wt, :], pvv.ap[:, 0:256], [pvv.key], [vR.key + ":%d" % wt])
                        if own:
                            pg = pb[4 + (j % 2)]
                            for kc in range(KC):
                                MM(pg.ap[:, 0:256], xb.ap[:, kc, j * 128:(j + 1) * 128], W.ap[:, kc, 512:768], kc == 0, kc == KC - 1,
                                   [W.key, xb.key], [pg.key])
                            ACT(sg.ap[:, ob * 4 + j, :], pg.ap[:, 0:256], AF.Silu, [pg.key], [sg.key + ":%d" % (ob * 4 + j)])
                    ptk = pb[4 + (tb % 2)] if not own else pb[6 + (tb % 2)]
                    ptkv = ptk.ap.bitcast(BF16).rearrange("p (a b) -> p a b", a=8)
                    for j in range(4 if 't' not in _sub else 0):
                        wt = tb * 4 + j
                        TR(ptkv[:, j, :], krT.ap[:, wt * 128:(wt + 1) * 128], identb, [krT.key + ":%d" % tb] + CKB, [ptk.key])
                        TS(ktm.ap[:, wt, :], ptkv[:, j, :], kd.ap[:, h, wt:wt + 1], None, ALU.mult, None, [ptk.key, kd.key],
                           [ktm.key + ":%d" % wt])
                import os as _os
                _rc = int(_os.environ.get('RET_CUT', '9'))
                if _rc < 2:
                    continue
                pS = pb[0]
                npre = NWT // 2
                for wt in range(npre):
                    MM(pS.ap[:, 0:256], ktm.ap[:, wt, :], vR.ap[:, wt, :], wt == 0, wt == npre - 1,
                       [ktm.key + ":%d" % wt, vR.key + ":%d" % wt], [pS.key])
                CP("scalar", S.ap, pS.ap[:, 0:256], [pS.key], [S.key])
                CP("vector", Sb.ap, S.ap, [S.key], [Sb.key])
                g128 = gam[h] ** 128
                if _rc < 3:
                    continue
                for b_ in range(NTT):
                    wt = npre + b_
                    ob = b_ // 4
                    pi = pb[1 + (b_ % 2)]
                    MM(pi.ap[:, 0:128], krT.ap[:, wt * 128:(wt + 1) * 128], qrT.ap[:, b_ * 128:(b_ + 1) * 128], True, True,
                       [krT.key + ":%d" % (wt // 4), qrT.key + ":%d" % ob], [pi.key])
                    at = AT[b_ % 2]
                    ti = tis[b_ % 2]
                    CP("scalar", ti.ap, pi.ap[:, 0:128], [pi.key], [ti.key])
                    TT(at.ap, ti.ap, mt.ap[:, h, :], ALU.mult, [ti.key, mt.key], [at.key])
                    py = pb[3 + (b_ % 2)]
                    MM(py.ap[:, 0:256], at.ap, vR.ap[:, wt, :], True, False, [at.key, vR.key + ":%d" % wt], [py.key])
                    MM(py.ap[:, 0:256], qtT.ap[:, b_ * 128:(b_ + 1) * 128], Sb.ap, False, True, [qtT.key + ":%d" % ob, Sb.key], [py.key])
                    if b_ < NTT - 1:
                        pkv = pb[5]
                        MM(pkv.ap[:, 0:256], ktm.ap[:, wt, :], vR.ap[:, wt, :], True, True,
                           [ktm.key + ":%d" % wt, vR.key + ":%d" % wt], [pkv.key])
                        CP("scalar", kvs.ap, pkv.ap[:, 0:256], [pkv.key], [kvs.key])
                        STT(S.ap, S.ap, g128, kvs.ap, ALU.mult, ALU.add, [S.key, kvs.key], [S.key])
                        CP("scalar", Sb.ap, S.ap, [S.key], [Sb.key])
                    if _rc < 4:
                        continue
                    ysb = ysbs[b_ % 2]
                    CP("scalar", ysb.ap, py.ap[:, 0:256], [py.key], [ysb.key])
                    P.add("vector", lambda e, o_=st6.ap, i_=ysb.ap: e.bn_stats(out=o_, in_=i_), [ysb.key], [st6.key])
                    P.add("vector", lambda e, o_=mv.ap, i_=st6.ap: e.bn_aggr(out=o_, in_=i_), [st6.key], [mv.key])
                    TS(rstd.ap, mv.ap[:, 1:2], EPS, None, ALU.add, None, [mv.key], [rstd.key])
                    ACT(rstd.ap, rstd.ap, AF.Sqrt, [rstd.key], [rstd.key])
                    RECIP(rstd.ap, rstd.ap, [rstd.key], [rstd.key])
                    y_ = yn[b_ % 2]
                    TS(y_.ap, ysb.ap, mv.ap[:, 0:1], rstd.ap, ALU.subtract, ALU.mult, [ysb.key, mv.key, rstd.key], [y_.key])
                    TT(y_.ap, y_.ap, gw.ap[:, h, :], ALU.mult, [y_.key, gw.key], [y_.key])
                    r_ = rr[b_ % 2]
                    TT(r_.ap, y_.ap, sg.ap[:, b_, :], ALU.mult, [y_.key, sg.key + ":%d" % b_], [r_.key])
                    ptr = pb[6 + (b_ % 2)]
                    ptrv = ptr.ap.bitcast(BF16).rearrange("p (a b) -> p a b", a=8)
                    for jj in range(2):
                        TR(ptrv[:, jj, :], r_.ap[:, jj * 128:(jj + 1) * 128], identb, [r_.key] + CKB, [ptr.key])
                    CP("scalar", rT.ap[:, :, b_ * 128:(b_ + 1) * 128], ptrv[:, 0:2, :], [ptr.key], [rT.key])
                for jj in range(2):
                    for tb in range(NTB):
                        DMA("sync", mixT_d[tb, :, HA + 2 * h + jj, :], rT.ap[:, jj, tb * 512:(tb + 1) * 512], [rT.key], [mixT_d.tensor.name])

        def phase_linear_A(actT_d, w_ap, Dout, res_d, dst_d):
            A.reset()
            act = [A.alloc("act%d" % i, [KC, 512], BF16) for i in range(NTB)]
            wb = [A.alloc("wb%d" % i, [KC, 512], BF16) for i in range(2)]
            rt = [A.alloc("rt%d" % i, [512], F32) for i in range(3)]
            ot = [A.alloc("ot%d" % i, [512], F32) for i in range(3)]
            for tb in range(NTB):
                DMA("sync", act[tb].ap, actT_d[tb], [actT_d.tensor.name], [act[tb].key])
            ncb = Dout // 512
            load_w(wb[0], w_ap, 0, 512, KC)
            cnt = 0
            for cb in range(ncb):
                w = wb[cb % 2]
                if cb + 1 < ncb:
                    load_w(wb[(cb + 1) % 2], w_ap, (cb + 1) * 512, (cb + 2) * 512, KC)
                for tt in range(NTT):
                    s = cnt % 3
                    pbk = pb[cnt % 4]
                    cnt += 1
                    DMA("sync", rt[s].ap, res_d[tt * 128:(tt + 1) * 128, cb * 512:(cb + 1) * 512], [res_d.tensor.name], [rt[s].key])
                    a = act[tt // 4]
                    for kc in range(KC):
                        MM(pbk.ap, a.ap[:, kc, (tt % 4) * 128:(tt % 4 + 1) * 128], w.ap[:, kc, :], kc == 0, kc == KC - 1,
                           [a.key, w.key], [pbk.key])
                    CP("scalar", ot[s].ap, pbk.ap, [pbk.key], [ot[s].key])
                    TT(ot[s].ap, ot[s].ap, rt[s].ap, ALU.add, [ot[s].key, rt[s].key], [ot[s].key])
                    DMA("sync", dst_d[tt * 128:(tt + 1) * 128, cb * 512:(cb + 1) * 512], ot[s].ap, [ot[s].key], [dst_d.tensor.name])

        def phase_xattn():
            A.reset()
            hn = [A.alloc("hn%d" % i, [KC, 512], BF16) for i in range(NTB)]
            wb = [A.alloc("wb%d" % i, [KC, 512], BF16) for i in range(2)]
            kTx = A.alloc("kTx", [KC, MEM], BF16)
            vX = A.alloc("vX", [2, D], BF16)
            mark = A.off
            mT = A.alloc("mT", [KC, MEM], BF16)
            DMA("sync", mT.ap, memT_d[0], [memT_d.tensor.name], [mT.key])
            for tb in range(NTB):
                DMA("sync", hn[tb].ap, hn2T_d[tb], [hn2T_d.tensor.name], [hn[tb].key])
            wcnt = 0
            bcnt = 0
            for cb in range(D // 512):
                w = wb[wcnt % 2]
                wcnt += 1
                load_w(w, wkv, cb * 512, (cb + 1) * 512, KC)
                for c4 in range(4):
                    pk = pb[bcnt % 4]
                    bcnt += 1
                    for kc in range(KC):
                        MM(pk.ap[:, 0:MEM], w.ap[:, kc, c4 * 128:(c4 + 1) * 128], mT.ap[:, kc, :], kc == 0, kc == KC - 1,
                           [w.key, mT.key], [pk.key])
                    CP(evac_eng(), kTx.ap[:, cb * 4 + c4, :], pk.ap[:, 0:MEM], [pk.key], [kTx.key])
            for cb in range(D // 512):
                w = wb[wcnt % 2]
                wcnt += 1
                load_w(w, wkv, D + cb * 512, D + (cb + 1) * 512, KC)
                for mtile in range(2):
                    pvv = pb[bcnt % 4]
                    bcnt += 1
                    for kc in range(KC):
                        MM(pvv.ap, mT.ap[:, kc, mtile * 128:(mtile + 1) * 128], w.ap[:, kc, :], kc == 0, kc == KC - 1,
                           [w.key, mT.key], [pvv.key])
                    CP(evac_eng(), vX.ap[:, mtile, cb * 512:(cb + 1) * 512], pvv.ap, [pvv.key], [vX.key])
            P.barrier()
            A.off = mark
            A.gen += 1
            qTx = A.alloc("qTx", [CX, T], BF16)
            PTx = [A.alloc("PTx%d" % i, [512], BF16) for i in range(2)]
            rcx = A.alloc("rcx", [512], F32)
            oTx = [A.alloc("oTx%d" % i, [512], BF16) for i in range(3)]
            otmp = A.alloc("otmp", [512], F32)
            sc = DX ** -0.5
            ocnt = 0
            for hx in range(XH):
                for cb2 in range(DX // 512):
                    w = wb[wcnt % 2]
                    wcnt += 1
                    c0 = hx * DX + cb2 * 512
                    load_w(w, wq, c0, c0 + 512, KC)
                    for tb in range(NTB):
                        for c4 in range(4):
                            pq = pb[bcnt % 4]
                            bcnt += 1
                            for kc in range(KC):
                                MM(pq.ap, w.ap[:, kc, c4 * 128:(c4 + 1) * 128], hn[tb].ap[:, kc, :], kc == 0, kc == KC - 1,
                                   [w.key, hn[tb].key], [pq.key])
                            TS(qTx.ap[:, cb2 * 4 + c4, tb * 512:(tb + 1) * 512], pq.ap, sc, None, ALU.mult, None, [pq.key], [qTx.key])
                for tb in range(NTB):
                    for kt in range(2):
                        ps = pb[4 + kt]
                        for c in range(CX):
                            MM(ps.ap, kTx.ap[:, hx * CX + c, kt * 128:(kt + 1) * 128], qTx.ap[:, c, tb * 512:(tb + 1) * 512],
                               c == 0, c == CX - 1, [kTx.key, qTx.key], [ps.key])
                        ACT(PTx[kt].ap, ps.ap, AF.Exp, [ps.key], [PTx[kt].key])
                    pd = pb[6]
                    for kt in range(2):
                        MM(pd.ap, onesb, PTx[kt].ap, kt == 0, kt == 1, [PTx[kt].key] + CKB, [pd.key])
                    CP("scalar", rcx.ap, pd.ap, [pd.key], [rcx.key])
                    RECIP(rcx.ap, rcx.ap, [rcx.key], [rcx.key])
                    for c in range(CX):
                        po = pb[bcnt % 4]
                        bcnt += 1
                        fc = hx * CX + c
                        for kt in range(2):
                            MM(po.ap, vX.ap[:, kt, fc * 128:(fc + 1) * 128], PTx[kt].ap, kt == 0, kt == 1,
                               [vX.key, PTx[kt].key], [po.key])
                        o = oTx[ocnt % 3]
                        ocnt += 1
                        CP("scalar", otmp.ap, po.ap, [po.key], [otmp.key])
                        TT(o.ap, otmp.ap, rcx.ap, ALU.mult, [otmp.key, rcx.key], [o.key])
                        DMA("sync", oT_d[tb, :, fc, :], o.ap, [o.key], [oT_d.tensor.name])

        def phase_peer_qr():
            A.reset()
            qpT = A.alloc("qpT", [16, T], BF16)
            sub = A.alloc("sub", [16, 128], BF16)
            mark = A.off
            hn = [A.alloc("hn%d" % i, [KC, 512], BF16) for i in range(NTB)]
            wb = [A.alloc("wb%d" % i, [KC, 512], BF16) for i in range(2)]
            DMA("gpsimd", sub.ap, subT.rearrange("p (a b) -> p a b", a=16), [], [sub.key])
            for tb in range(NTB):
                DMA("sync", hn[tb].ap, hn3T_d[tb], [hn3T_d.tensor.name], [hn[tb].key])
            bcnt = 0
            for cb in range(4):
                w = wb[cb % 2]
                load_w(w, pwq, cb * 512, (cb + 1) * 512, KC)
                for tb in range(NTB):
                    for c4 in range(4):
                        pq = pb[bcnt % 4]
                        bcnt += 1
                        for kc in range(KC):
                            MM(pq.ap, w.ap[:, kc, c4 * 128:(c4 + 1) * 128], hn[tb].ap[:, kc, :], kc == 0, kc == KC - 1,
                               [w.key, hn[tb].key], [pq.key])
                        CP(evac_eng(), qpT.ap[:, cb * 4 + c4, tb * 512:(tb + 1) * 512], pq.ap, [pq.key], [qpT.key])
            P.barrier()
            A.off = mark
            A.gen += 1
            qk = [qpT.key]
            s = A.alloc("s", [16, 128], F32)
            s2 = A.alloc("s2", [16, 128], F32)
            T16 = A.alloc("T16", [16, 16], F32)
            I16 = A.alloc("I16", [16, 16], U32)
            I16f = A.alloc("I16f", [16, 16], F32)
            cand = A.alloc("cand", [8, 256], F32)
            cand2 = A.alloc("cand2", [8, 256], F32)
            B16 = A.alloc("B16", [8, 16], F32)
            C16 = A.alloc("C16", [8, 16], U32)
            Ai = A.alloc("Ai", [8, 16], U32)
            Bi = A.alloc("Bi", [8, 16], U32)
            Af = A.alloc("Af", [8, 16], F32)
            Bf = A.alloc("Bf", [8, 16], F32)
            eb = A.alloc("eb", [8, 16], F32)
            Z = A.alloc("Z", [8], F32)
            eq = A.alloc("eq", [128, 16], F32)
            R = A.alloc("R", [3, 128], F32)
            RT = A.alloc("RT", [3, 128], F32)
            OI = A.alloc("OI", [128, 128], BF16)
            OJ = A.alloc("OJ", [128, 128], BF16)
            Gt = A.alloc("Gt", [128, 128], BF16)
            subv = sub.ap
            K = lambda *a: list(a)
            for tt in range(NTT):
                for hp in range(16):
                    bk = pb[hp // 4]
                    MM(bk.ap[:, (hp % 4) * 128:(hp % 4 + 1) * 128], qpT.ap[:, hp, tt * 128:(tt + 1) * 128], subv[:, hp, :], True, True,
                       qk + [sub.key], [bk.key])
                for q in range(4):
                    CP(evac_eng(), s.ap[:, q * 4:(q + 1) * 4, :], pb[q].ap.rearrange("p (a b) -> p a b", a=4), [pb[q].key], [s.key])
                for hp in range(16):
                    sv = s.ap[:, hp, :]
                    s2v = s2.ap[:, hp, :]
                    P.add("vector", lambda e, o=T16.ap[:, hp, 0:8], i=sv: e.max(out=o, in_=i), [s.key], [T16.key])
                    P.add("vector", lambda e, o=I16.ap[:, hp, 0:8], m=T16.ap[:, hp, 0:8], i=sv: e.max_index(out=o, in_max=m, in_values=i),
                          [s.key, T16.key], [I16.key])
                    P.add("vector", lambda e, o=s2v, m=T16.ap[:, hp, 0:8], i=sv: e.match_replace(out=o, in_to_replace=m, in_values=i, imm_value=NEG),
                          [s.key, T16.key], [s2.key])
                    P.add("vector", lambda e, o=T16.ap[:, hp, 8:16], i=s2v: e.max(out=o, in_=i), [s2.key], [T16.key])
                    P.add("vector", lambda e, o=I16.ap[:, hp, 8:16], m=T16.ap[:, hp, 8:16], i=s2v: e.max_index(out=o, in_max=m, in_values=i),
                          [s2.key, T16.key], [I16.key])
                CP("vector", I16f.ap, I16.ap, [I16.key], [I16f.key])
                T16v = T16.ap.rearrange("p (h two) k -> p h two k", two=2)
                I16v = I16f.ap.rearrange("p (h two) k -> p h two k", two=2)
                candv = cand.ap.rearrange("p h (a b) -> p h a b", a=16)
                TT(candv, T16v[:, :, 0, :].unsqueeze(3).broadcast_to([128, 8, 16, 16]),
                   T16v[:, :, 1, :].unsqueeze(2).broadcast_to([128, 8, 16, 16]), ALU.add, [T16.key], [cand.key])
                for h in range(8):
                    cv = cand.ap[:, h, :]
                    c2v = cand2.ap[:, h, :]
                    P.add("vector", lambda e, o=B16.ap[:, h, 0:8], i=cv: e.max(out=o, in_=i), [cand.key], [B16.key])
                    P.add("vector", lambda e, o=C16.ap[:, h, 0:8], m=B16.ap[:, h, 0:8], i=cv: e.max_index(out=o, in_max=m, in_values=i),
                          [cand.key, B16.key], [C16.key])
                    P.add("vector", lambda e, o=c2v, m=B16.ap[:, h, 0:8], i=cv: e.match_replace(out=o, in_to_replace=m, in_values=i, imm_value=NEG),
                          [cand.key, B16.key], [cand2.key])
                    P.add("vector", lambda e, o=B16.ap[:, h, 8:16], i=c2v: e.max(out=o, in_=i), [cand2.key], [B16.key])
                    P.add("vector", lambda e, o=C16.ap[:, h, 8:16], m=B16.ap[:, h, 8:16], i=c2v: e.max_index(out=o, in_max=m, in_values=i),
                          [cand2.key, B16.key], [C16.key])
                TT(eb.ap, B16.ap, B16.ap[:, :, 0:1].broadcast_to([128, 8, 16]), ALU.subtract, [B16.key], [eb.key])
                ACT(eb.ap, eb.ap, AF.Exp, [eb.key], [eb.key])
                P.add("vector", lambda e, o=Z.ap, i=eb.ap: e.tensor_reduce(out=o, in_=i, axis=AX.X, op=ALU.add), [eb.key], [Z.key])
                RECIP(Z.ap, Z.ap, [Z.key], [Z.key])
                TT(R.ap[:, 2, :].rearrange("p (h k) -> p h k", h=8), eb.ap, Z.ap.unsqueeze(2).broadcast_to([128, 8, 16]), ALU.mult,
                   [eb.key, Z.key], [R.key + ":2"])
                P.add("vector", lambda e, o=Ai.ap, i=C16.ap: e.tensor_single_scalar(out=o, in_=i, scalar=4, op=ALU.logical_shift_right),
                      [C16.key], [Ai.key])
                P.add("vector", lambda e, o=Bi.ap, i=C16.ap: e.tensor_single_scalar(out=o, in_=i, scalar=15, op=ALU.bitwise_and),
                      [C16.key], [Bi.key])
                CP("vector", Af.ap, Ai.ap, [Ai.key], [Af.key])
                CP("vector", Bf.ap, Bi.ap, [Bi.key], [Bf.key])
                for r_i, (sel, half) in enumerate(((Af, 0), (Bf, 1))):
                    TT(eq.ap, iotaf[:, 0:16].unsqueeze(1).broadcast_to([128, 128, 16]),
                       sel.ap.rearrange("p h k -> p (h k)").unsqueeze(2).broadcast_to([128, 128, 16]), ALU.is_equal,
                       [sel.key] + CK, [eq.key])
                    eq4 = eq.ap.rearrange("p (h k) a -> p h k a", h=8)
                    TT(eq4, eq4, I16v[:, :, half, :].unsqueeze(2).broadcast_to([128, 8, 16, 16]), ALU.mult, [eq.key, I16f.key], [eq.key])
                    P.add("vector", lambda e, o=R.ap[:, r_i, :], i=eq.ap: e.tensor_reduce(out=o, in_=i, axis=AX.X, op=ALU.add),
                          [eq.key], [R.key + ":%d" % r_i])
                ptR = pb[4]
                for r_i in range(3):
                    TR(ptR.ap[:, r_i * 128:(r_i + 1) * 128], R.ap[:, r_i, :], identf, [R.key + ":%d" % r_i] + CK, [ptR.key])
                CP("scalar", RT.ap, ptR.ap[:, 0:384].rearrange("p (a b) -> p a b", a=3), [ptR.key], [RT.key])
                TT(OI.ap, iotaf.unsqueeze(1).broadcast_to([128, 128, 128]), RT.ap[:, 0, :].unsqueeze(2).broadcast_to([128, 128, 128]),
                   ALU.is_equal, [RT.key] + CK, [OI.key])
                TT(OJ.ap, iotaf.unsqueeze(1).broadcast_to([128, 128, 128]), RT.ap[:, 1, :].unsqueeze(2).broadcast_to([128, 128, 128]),
                   ALU.is_equal, [RT.key] + CK, [OJ.key])
                TT(OJ.ap, OJ.ap, RT.ap[:, 2, :].unsqueeze(2).broadcast_to([128, 128, 128]), ALU.mult, [OJ.key, RT.key], [OJ.key])
                for t4 in range(32):
                    pg = pb[5 + (t4 % 3)]
                    for u in range(4):
                        t = t4 * 4 + u
                        MM(pg.ap[:, u * 128:(u + 1) * 128], OJ.ap[:, t, :], OI.ap[:, t, :], True, True, [OJ.key, OI.key], [pg.key])
                    CP(evac_eng(), Gt.ap[:, :, t4 * 4:(t4 + 1) * 4], pg.ap.rearrange("p (t i) -> p i t", t=4), [pg.key], [Gt.key])
                for i0 in range(0, 128, 16):
                    DMA("sync", G_d[i0:i0 + 16, :, tt * 128:(tt + 1) * 128].rearrange("i j t -> j i t"), Gt.ap[:, i0:i0 + 16, :],
                        [Gt.key], [G_d.tensor.name])

        def phase_peer_u():
            A.reset()
            hn = [A.alloc("hn%d" % i, [KC, 512], BF16) for i in range(NTB)]
            wb = [A.alloc("wb%d" % i, [KC, 512], BF16) for i in range(2)]
            Gi = [A.alloc("Gi%d" % i, [T], BF16) for i in range(3)]
            Aa = [A.alloc("Aa%d" % i, [512], BF16) for i in range(2)]
            GA = [A.alloc("GA%d" % i, [T], BF16) for i in range(3)]
            for tb in range(NTB):
                DMA("sync", hn[tb].ap, hn3T_d[tb], [hn3T_d.tensor.name], [hn[tb].key])
            load_w(wb[0], uT, 0, 512, KC)
            bcnt = 0
            for ib4 in range(NE // 4):
                w = wb[ib4 % 2]
                if ib4 + 1 < NE // 4:
                    load_w(wb[(ib4 + 1) % 2], uT, (ib4 + 1) * 512, (ib4 + 2) * 512, KC)
                for e4 in range(4):
                    i = ib4 * 4 + e4
                    g = Gi[i % 3]
                    ga = GA[i % 3]
                    DMA("sync", g.ap, G_d[i], [G_d.tensor.name], [g.key])
                    for tb in range(NTB):
                        pa = pb[bcnt % 6]
                        a_ = Aa[bcnt % 2]
                        bcnt += 1
                        for kc in range(KC):
                            MM(pa.ap, w.ap[:, kc, e4 * 128:(e4 + 1) * 128], hn[tb].ap[:, kc, :], kc == 0, kc == KC - 1,
                               [w.key, hn[tb].key], [pa.key])
                        ACT(a_.ap, pa.ap, AF.Gelu, [pa.key], [a_.key])
                        TT(ga.ap[:, tb * 512:(tb + 1) * 512], a_.ap, g.ap[:, tb * 512:(tb + 1) * 512], ALU.mult, [a_.key, g.key], [ga.key])
                    DMA("sync", GA_d[i], ga.ap, [ga.key], [GA_d.tensor.name])

        def phase_peer_v():
            A.reset()
            EG = 8
            vb = [A.alloc("vb%d" % i, [EG, 512], BF16) for i in range(2)]
            gg = [A.alloc("gg%d" % i, [EG, T], BF16) for i in range(2)]
            rt = [A.alloc("rt%d" % i, [512], F32) for i in range(2)]
            ot = [A.alloc("ot%d" % i, [512], F32) for i in range(2)]
            ng = NE // EG
            cnt = 0
            for cb in range(D // 512):
                for ig in range(ng):
                    v_ = vb[cnt % 2]
                    g_ = gg[cnt % 2]
                    cnt += 1
                    DMA("gpsimd", v_.ap, pv[ig * EG * 128:(ig + 1) * EG * 128, cb * 512:(cb + 1) * 512].rearrange("(e p) n -> p e n", p=128),
                        [], [v_.key])
                    DMA("sync", g_.ap, GA_d[ig * EG:(ig + 1) * EG].rearrange("e p t -> p e t"), [GA_d.tensor.name], [g_.key])
                    for e_ in range(EG):
                        first = (ig == 0 and e_ == 0)
                        last = (ig == ng - 1 and e_ == EG - 1)
                        for tt in range(NTT):
                            MM(pb[tt].ap, g_.ap[:, e_, tt * 128:(tt + 1) * 128], v_.ap[:, e_, :], first, last, [g_.key, v_.key], [pb[tt].key])
                for tt in range(NTT):
                    s = tt % 2
                    DMA("sync", rt[s].ap, h2_d[tt * 128:(tt + 1) * 128, cb * 512:(cb + 1) * 512], [h2_d.tensor.name], [rt[s].key])
                    CP("scalar", ot[s].ap, pb[tt].ap, [pb[tt].key], [ot[s].key])
                    TT(ot[s].ap, ot[s].ap, rt[s].ap, ALU.add, [ot[s].key, rt[s].key], [ot[s].key])
                    DMA("sync", h3_d[tt * 128:(tt + 1) * 128, cb * 512:(cb + 1) * 512], ot[s].ap, [ot[s].key], [h3_d.tensor.name])

        sched = [
            lambda: phase_nt(xw, W2, n1w, xnT_d, 512),
            phase_attn,
            phase_ret,
            lambda: phase_linear_A(mixT_d, w_out, D, xw[T:W2, :], h1_d),
            lambda: phase_nt(h1_d, T, n2w, hn2T_d, 512),
            lambda: phase_nt(memb, MEM, nmw, memT_d, MEM),
            phase_xattn,
            lambda: phase_linear_A(oT_d, wo, D, h1_d, h2_d),
            lambda: phase_nt(h2_d, T, n3w, hn3T_d, 512),
            phase_peer_qr,
            phase_peer_u,
            phase_peer_v,
            lambda: phase_nt(h3_d, T, nfw, None, 512, final_out=out),
        ]
        for i, ph in enumerate(sched):
            if nph is not None and i >= nph:
                break
            if i:
                P.barrier()
            ph()
        P.emit()
    return nc


def _host_tables(D, T, half):
    W2 = 2 * T
    NWT = W2 // 128
    HR = D // 512
    pos = np.arange(-T, T, dtype=np.float64) + half * T
    inv_freq = 1.0 / (10000.0 ** (np.arange(0, 128, 2, dtype=np.float64) / 128.0))
    ang = pos[None, :] * inv_freq[:, None]
    cos = np.cos(ang)
    sin = np.sin(ang)
    costab = np.concatenate([cos, cos], 0).astype(np.float32)
    sintab = np.concatenate([-sin, sin], 0).astype(np.float32)
    lg = np.log1p(-np.power(2.0, -5.0 - np.arange(HR, dtype=np.float64)))
    p = np.arange(128, dtype=np.float64)
    mk = p[:, None]
    cq = p[None, :]
    valid = (mk // 64) <= (cq // 64)
    mtab = np.zeros((128, HR, 128), np.float64)
    gtab = np.zeros((128, HR, 128), np.float64)
    kdec = np.zeros((128, HR, NWT), np.float64)
    for h in range(HR):
        mtab[:, h, :] = np.where(valid, np.exp(lg[h] * np.abs(cq - mk)), 0.0) * (128 ** -0.5)
        gtab[:, h, :] = np.exp(lg[h] * (cq + 1.0))
        for wt in range(NWT):
            if wt < NWT // 2:
                t = wt * 128 + p
                kdec[:, h, wt] = np.exp(lg[h] * (T - 1 - t)) * (128 ** -0.5)
            else:
                kdec[:, h, wt] = np.exp(lg[h] * (127 - p)) * (128 ** -0.5)
    return (costab, sintab, mtab.reshape(128, -1).astype(np.float32), gtab.reshape(128, -1).astype(np.float32),
            kdec.reshape(128, -1).astype(np.float32))


def _rel_bias_table(rb):
    mk = np.arange(128)[:, None]
    col = np.arange(640)[None, :]
    jq = col // 128
    cq = col % 128
    dist = 128 * jq + cq - mk
    idx = np.clip(dist, -256, 256) + 256
    diff = 2 * jq + (cq // 64) - (mk // 64)
    valid = (diff >= 0) & (diff <= 8)
    tab = rb[:, idx]
    return np.where(valid[None], tab, np.float32(NEG)).astype(np.float32)


_CACHE = {}


def _get_prog(D, T):
    key = (D, T)
    if key not in _CACHE:
        _CACHE[key] = build_program(D, T)
    return _CACHE[key]


def kernel(x, mem, norm1_w, w_in, attn_rel_bias, ret_gn_w, w_out, norm2_w, mem_norm_w,
           xattn_wq, xattn_wkv, xattn_wo, norm3_w, peer_wq, peer_subkeys, peer_u, peer_v,
           final_norm_w):
    f = lambda a: np.ascontiguousarray(np.asarray(a, dtype=np.float32))
    x = f(x)
    mem = f(mem)
    B, S, D = x.shape
    T = S // 2
    HA = D // 256
    HR = D // 512
    AW = HA * 128
    win = f(w_in)[0]
    aq, ak, av = win[:, 0:AW], win[:, AW:2 * AW], win[:, 2 * AW:3 * AW]
    o = 3 * AW
    rq, rk = win[:, o:o + HR * 128], win[:, o + HR * 128:o + 2 * HR * 128]
    o2 = o + 2 * HR * 128
    rv, rg = win[:, o2:o2 + HR * 256], win[:, o2 + HR * 256:o2 + 2 * HR * 256]
    w_attn = np.concatenate([np.concatenate([aq[:, h * 128:(h + 1) * 128], ak[:, h * 128:(h + 1) * 128], av[:, h * 128:(h + 1) * 128]], 1)
                             for h in range(HA)], 1)
    w_ret = np.concatenate([np.concatenate([rq[:, h * 128:(h + 1) * 128], rk[:, h * 128:(h + 1) * 128],
                                            rv[:, h * 256:(h + 1) * 256], rg[:, h * 256:(h + 1) * 256]], 1) for h in range(HR)], 1)
    rep = lambda v: np.ascontiguousarray(np.broadcast_to(f(v).reshape(1, -1), (128, f(v).size)))
    relbT = _rel_bias_table(f(attn_rel_bias)[0])
    sub = f(peer_subkeys)[0]
    subT = np.ascontiguousarray(sub.transpose(3, 0, 1, 2).reshape(128, 16 * 128))
    uT = np.ascontiguousarray(f(peer_u)[0].T)
    consts = np.zeros((128, 512), np.float32)
    consts[:, 0:128] = np.eye(128, dtype=np.float32)
    consts[:, 128:256] = np.roll(np.eye(128, dtype=np.float32), 64, axis=0)
    consts[:, 256:384] = 1.0
    consts[:, 384:512] = np.arange(128, dtype=np.float32)[None, :]
    shared = dict(
        n1w=rep(norm1_w), n2w=rep(norm2_w), nmw=rep(mem_norm_w), n3w=rep(norm3_w), nfw=rep(final_norm_w),
        w_attn=np.ascontiguousarray(w_attn), w_ret=np.ascontiguousarray(w_ret), relbT=relbT,
        gnw=rep(ret_gn_w), w_out=f(w_out)[0], wq=f(xattn_wq)[0], wkv=f(xattn_wkv)[0], wo=f(xattn_wo)[0],
        pwq=f(peer_wq)[0], subT=subT, uT=uT, pv=f(peer_v)[0], consts=consts,
    )
    NKP = (1 + T // 512) * 4
    in_maps = []
    for b in range(B):
        for half in range(2):
            xw = np.zeros((2 * T, D), np.float32)
            if half == 1:
                xw[:T] = x[b, :T]
            xw[T:] = x[b, half * T:(half + 1) * T]
            km = np.zeros((128, NKP), np.float32)
            if half == 0:
                km[:, 0:4] = NEG
            costab, sintab, mtab, gtab, kdec = _host_tables(D, T, half)
            m = dict(shared)
            m.update(xw=xw, memb=mem[b], kmask=km, costab=costab, sintab=sintab, mtab=mtab, gtab=gtab, kdec=kdec)
            in_maps.append(m)
    nc = _get_prog(D, T)
    res = run_bass_kernel_spmd(nc, in_maps, core_ids=list(range(len(in_maps))))
    outp = np.zeros((B, S, D), np.float32)
    for b in range(B):
        for half in range(2):
            outp[b, half * T:(half + 1) * T] = res.results[b * 2 + half]["out"]
    return outp
```

```python
import contextlib
import math
import numpy as np
import concourse.bass as bass
import concourse.mybir as mybir
from concourse.bass_utils import run_bass_kernel_spmd

F32 = mybir.dt.float32
BF16 = mybir.dt.bfloat16
U32 = mybir.dt.uint32
AF = mybir.ActivationFunctionType
ALU = mybir.AluOpType
AX = mybir.AxisListType

EPS = 1e-6
NEG = -1e30
ENGS = ("tensor", "vector", "scalar", "gpsimd", "sync")


class Op:
    __slots__ = ("eng", "fn", "waits", "signal", "seq", "is_dma", "dsem", "dval", "pos", "ewaits")

    def __init__(self, eng, fn, is_dma):
        self.eng = eng
        self.fn = fn
        self.is_dma = is_dma
        self.waits = []
        self.signal = False
        self.seq = None
        self.dsem = None
        self.dval = None


class Prog:
    def __init__(self, nc, n_dma_sems=24):
        self.nc = nc
        self.ops = {e: [] for e in ENGS}
        self.last_w = {}
        self.readers = {}
        self.n_dma_sems = n_dma_sems
        self.dma_rr = {e: 0 for e in ENGS}
        self.dma_last = {}
        self.dma_cnt = {}
        self.pending_barrier = {e: None for e in ENGS}

    def barrier(self):
        b = []
        for e in ENGS:
            for op in reversed(self.ops[e]):
                if not op.is_dma:
                    b.append(op)
                    break
        b.extend(self.dma_last.values())
        for e in ENGS:
            self.pending_barrier[e] = b
        self.last_w = {}
        self.readers = {}

    def add(self, eng, fn, reads=(), writes=(), dma=False):
        op = Op(eng, fn, dma)
        w = op.waits
        pb = self.pending_barrier[eng]
        if pb is not None:
            w.extend(p for p in pb if p is not op)
            self.pending_barrier[eng] = None
        for k in reads:
            p = self.last_w.get(k)
            if p is not None:
                w.append(p)
        for k in writes:
            p = self.last_w.get(k)
            if p is not None:
                w.append(p)
            r = self.readers.get(k)
            if r:
                w.extend(r)
        for k in reads:
            self.readers.setdefault(k, []).append(op)
        for k in writes:
            self.last_w[k] = op
            self.readers[k] = []
        if eng == "tensor":
            op.waits = w = [p for p in w if p.eng != "tensor" or p.is_dma]
        if dma:
            slot = self.dma_rr[eng]
            self.dma_rr[eng] = (slot + 1) % self.n_dma_sems
            prev = self.dma_last.get((eng, slot))
            if prev is not None:
                w.append(prev)
            self.dma_last[(eng, slot)] = op
            c = self.dma_cnt.get((eng, slot), 0) + 1
            self.dma_cnt[(eng, slot)] = c
            op.dsem = (eng, slot)
            op.dval = 16 * c
        self.ops[eng].append(op)
        return op

    def emit(self):
        nc = self.nc
        for e in ENGS:
            for i, op in enumerate(self.ops[e]):
                op.pos = i
        for e in ENGS:
            for op in self.ops[e]:
                best = {}
                for p in op.waits:
                    if not p.is_dma:
                        b = best.get(p.eng)
                        if b is None or p.pos > b.pos:
                            best[p.eng] = p
                op.ewaits = list(best.values())
                for p in op.ewaits:
                    p.signal = True
        for e in ENGS:
            n = 0
            for op in self.ops[e]:
                if op.signal and not op.is_dma:
                    n += 1
                    op.seq = n
        with contextlib.ExitStack() as st:
            esem = {e: st.enter_context(nc.semaphore("es_" + e)) for e in ENGS}
            dsem = {}
            for e in ENGS:
                if any(o.is_dma for o in self.ops[e]):
                    for s in range(self.n_dma_sems):
                        dsem[(e, s)] = st.enter_context(nc.semaphore("ds_%s_%d" % (e, s)))
            block = st.enter_context(nc.Block())

            def run(eng_name):
                def body(eng):
                    known_e = {}
                    known_d = {}
                    for op in self.ops[eng_name]:
                        need_e = {}
                        need_d = {}
                        for p in op.waits:
                            if p.is_dma:
                                if need_d.get(p.dsem, 0) < p.dval:
                                    need_d[p.dsem] = p.dval
                        for p in op.ewaits:
                            need_e[p.eng] = p.seq
                        for k, v in need_e.items():
                            if known_e.get(k, 0) < v:
                                eng.wait_ge(esem[k], v)
                                known_e[k] = v
                        for k, v in need_d.items():
                            if known_d.get(k, 0) < v:
                                eng.wait_ge(dsem[k], v)
                                known_d[k] = v
                        ins = op.fn(eng)
                        if op.is_dma:
                            ins.then_inc(dsem[op.dsem], 16)
                        elif op.signal:
                            ins.then_inc(esem[eng_name], 1)
                    if eng_name == "sync":
                        for (e, s), c in self.dma_cnt.items():
                            eng.wait_ge(dsem[(e, s)], 16 * c)
                return body

            for e in ENGS:
                if self.ops[e] or e == "sync":
                    getattr(block, e)(run(e))


class Tl:
    __slots__ = ("ap", "key")

    def __init__(self, ap, key):
        self.ap = ap
        self.key = key

    def __getitem__(self, idx):
        return self.ap[idx]


_DT_BYTES = {F32: 4, BF16: 2, U32: 4}


class Arena:
    def __init__(self, nc, st, nbytes):
        self.t = st.enter_context(nc.sbuf_tensor("arena", [128, nbytes // 4], F32))
        self.words = nbytes // 4
        self.off = 0
        self.gen = 0

    def reset(self):
        self.off = 0
        self.gen += 1

    def alloc(self, name, free_shape, dt):
        n = 1
        for s in free_shape:
            n *= s
        words = (n * _DT_BYTES[dt] + 3) // 4
        words = (words + 7) // 8 * 8
        assert self.off + words <= self.words, "arena overflow at %s: %d + %d > %d" % (name, self.off * 4, words * 4, self.words * 4)
        ap = self.t[:, self.off:self.off + words]
        self.off += words
        if dt != F32:
            ap = ap.bitcast(dt)
        ap = ap[:, 0:n]
        if len(free_shape) == 2:
            ap = ap.rearrange("p (a b) -> p a b", a=free_shape[0])
        elif len(free_shape) == 3:
            ap = ap.rearrange("p (a b c) -> p a b c", a=free_shape[0], b=free_shape[1])
        elif len(free_shape) == 4:
            ap = ap.rearrange("p (a b c d) -> p a b c d", a=free_shape[0], b=free_shape[1], c=free_shape[2])
        return Tl(ap, "%s#%d" % (name, self.gen))


def build_program(D, T, nph=None, dbg=False, stub_peer=False):
    KC = D // 128
    W2 = 2 * T
    NTB = T // 512
    NWB = W2 // 512
    NTT = T // 128
    NWT = W2 // 128
    HA = D // 256
    HR = D // 512
    NAB = 1 + NTB
    NKP = NAB * 4
    XH = 4
    DX = D // XH
    CX = DX // 128
    MEM = 256
    NE = 128
    gam = [1.0 - 2.0 ** (-5.0 - h) for h in range(HR)]

    nc = bass.Bass("TRN2", target_bir_lowering=False)

    def din(name, shape, dt=F32):
        return nc.dram_tensor(name, list(shape), dt, kind="ExternalInput").ap()

    def dscr(name, shape, dt):
        if dbg:
            return nc.dram_tensor(name, list(shape), dt, kind="ExternalOutput").ap()
        return nc.dram_tensor(name, list(shape), dt).ap()

    xw = din("xw", [W2, D])
    memb = din("memb", [MEM, D])
    n1w = din("n1w", [128, D])
    n2w = din("n2w", [128, D])
    nmw = din("nmw", [128, D])
    n3w = din("n3w", [128, D])
    nfw = din("nfw", [128, D])
    w_attn = din("w_attn", [D, HA * 384])
    w_ret = din("w_ret", [D, HR * 768])
    relbT = din("relbT", [HA, 128, 640])
    kmask = din("kmask", [128, NKP])
    costab = din("costab", [128, W2])
    sintab = din("sintab", [128, W2])
    mtab = din("mtab", [128, HR * 128])
    gtab = din("gtab", [128, HR * 128])
    kdec = din("kdec", [128, HR * NWT])
    gnw = din("gnw", [128, HR * 256])
    w_out = din("w_out", [D, D])
    wq = din("wq", [D, D])
    wkv = din("wkv", [D, 2 * D])
    wo = din("wo", [D, D])
    pwq = din("pwq", [D, 2048])
    subT = din("subT", [128, 16 * 128])
    uT = din("uT", [D, NE * 128] if not stub_peer else [128, 128])
    pv = din("pv", [NE * 128, D] if not stub_peer else [128, 128])
    consts = din("consts", [128, 4 * 128])
    out = nc.dram_tensor("out", [T, D], F32, kind="ExternalOutput").ap()

    xnT_d = dscr("xnT_d", [NWB, 128, KC, 512], BF16)
    mixT_d = dscr("mixT_d", [NTB, 128, KC, 512], BF16)
    h1_d = dscr("h1_d", [T, D], F32)
    hn2T_d = dscr("hn2T_d", [NTB, 128, KC, 512], BF16)
    memT_d = dscr("memT_d", [1, 128, KC, MEM], BF16)
    oT_d = dscr("oT_d", [NTB, 128, KC, 512], BF16)
    h2_d = dscr("h2_d", [T, D], F32)
    hn3T_d = dscr("hn3T_d", [NTB, 128, KC, 512], BF16)
    G_d = dscr("G_d", [NE, 128, T], BF16)
    GA_d = dscr("GA_d", [NE, 128, T], BF16)
    h3_d = dscr("h3_d", [T, D], F32)

    P = Prog(nc)
    st = contextlib.ExitStack()
    with st:
        A = Arena(nc, st, 200 * 1024)
        pb = [Tl(st.enter_context(nc.psum_tensor("pb%d" % i, [128, 512], F32))[:], "pb%d" % i) for i in range(8)]
        cst_f = st.enter_context(nc.sbuf_tensor("cst_f", [128, 512], F32))
        cst_b = st.enter_context(nc.sbuf_tensor("cst_b", [128, 512], BF16))
        identf = cst_f[:, 0:128]
        iotaf = cst_f[:, 384:512]
        identb = cst_b[:, 0:128]
        permb = cst_b[:, 128:256]
        onesb = cst_b[:, 256:384]
        CK = ["cst"]

        def DMA(eng, o, i, reads, writes):
            P.add(eng, lambda e: e.dma_start(out=o, in_=i), reads, writes, dma=True)

        def MM(o, lhsT, rhs, start, stop, reads, writes):
            P.add("tensor", lambda e: e.matmul(o, lhsT=lhsT, rhs=rhs, start=start, stop=stop), reads, writes)

        def TR(o, i, ident, reads, writes):
            P.add("tensor", lambda e: e.transpose(out=o, in_=i, identity=ident), reads, writes)

        def ACT(o, i, func, reads, writes, bias=None, scale=None, accum=None):
            kw = {}
            if bias is not None:
                kw["bias"] = bias
            if scale is not None:
                kw["scale"] = scale
            if accum is not None:
                kw["accum_out"] = accum
            P.add("scalar", lambda e: e.activation(out=o, in_=i, func=func, **kw), reads, writes)

        def CP(eng, o, i, reads, writes):
            if eng == "scalar":
                P.add("scalar", lambda e: e.activation(out=o, in_=i, func=AF.Copy), reads, writes)
            else:
                P.add(eng, lambda e: e.tensor_copy(out=o, in_=i), reads, writes)

        def TT(o, a, b, op, reads, writes, eng="vector"):
            P.add(eng, lambda e: e.tensor_tensor(out=o, in0=a, in1=b, op=op), reads, writes)

        def TS(o, a, s1, s2, op0, op1, reads, writes, eng="vector"):
            if op1 is None:
                P.add(eng, lambda e: e.tensor_scalar(out=o, in0=a, scalar1=s1, scalar2=None, op0=op0), reads, writes)
            else:
                P.add(eng, lambda e: e.tensor_scalar(out=o, in0=a, scalar1=s1, scalar2=s2, op0=op0, op1=op1), reads, writes)

        def STT(o, a, s, b, op0, op1, reads, writes):
            P.add("vector", lambda e: e.scalar_tensor_tensor(out=o, in0=a, scalar=s, in1=b, op0=op0, op1=op1), reads, writes)

        def RECIP(o, i, reads, writes):
            P.add("vector", lambda e: e.reciprocal(out=o, in_=i), reads, writes)

        def wview(w_ap, c0, c1):
            return w_ap[:, c0:c1].rearrange("(kc p) n -> p kc n", p=128)

        def load_w(dst, w_ap, c0, c1, kcn, reads=()):
            v = wview(w_ap, c0, c1)
            step = max(1, 1024 // 128)
            for k0 in range(0, kcn, step):
                k1 = min(kcn, k0 + step)
                for n0 in range(0, c1 - c0, 512):
                    n1 = min(c1 - c0, n0 + 512)
                    DMA("gpsimd", dst.ap[:, k0:k1, n0:n1], v[:, k0:k1, n0:n1], list(reads), [dst.key])

        evac_rr = [0]

        def evac_eng():
            evac_rr[0] += 1
            return "scalar" if evac_rr[0] % 2 else "vector"

        DMA("sync", cst_f[:], consts, [], CK)
        CP("vector", cst_b[:], cst_f[:], CK, ["cstb"])
        CKB = ["cstb"]

        def phase_nt(src, ntok, wrep_d, dst_d, blk, final_out=None):
            A.reset()
            wrep = A.alloc("wrep", [D], F32)
            xt = [A.alloc("xt%d" % i, [D], F32) for i in range(2)]
            junk = A.alloc("junk", [D], BF16)
            ss = [A.alloc("ss%d" % i, [1], F32) for i in range(2)]
            rs = [A.alloc("rs%d" % i, [1], F32) for i in range(2)]
            if final_out is None:
                xb = [A.alloc("xb%d" % i, [D], BF16) for i in range(2)]
                xT = [A.alloc("xT%d" % i, [KC, 128], BF16) for i in range(2)]
            else:
                xo = [A.alloc("xo%d" % i, [D], F32) for i in range(2)]
            DMA("sync", wrep.ap, wrep_d, [], [wrep.key])
            nt = ntok // 128
            per = blk // 128
            bank = 0
            for i in range(nt):
                s = i % 2
                DMA("sync", xt[s].ap, src[i * 128:(i + 1) * 128, :], [], [xt[s].key])
                ACT(junk.ap, xt[s].ap, AF.Square, [xt[s].key], [junk.key, ss[s].key], accum=ss[s].ap)
                TS(rs[s].ap, ss[s].ap, 1.0 / D, EPS, ALU.mult, ALU.add, [ss[s].key], [rs[s].key])
                ACT(rs[s].ap, rs[s].ap, AF.Sqrt, [rs[s].key], [rs[s].key])
                RECIP(rs[s].ap, rs[s].ap, [rs[s].key], [rs[s].key])
                if final_out is not None:
                    STT(xo[s].ap, xt[s].ap, rs[s].ap, wrep.ap, ALU.mult, ALU.mult, [xt[s].key, rs[s].key, wrep.key], [xo[s].key])
                    DMA("sync", final_out[i * 128:(i + 1) * 128, :], xo[s].ap, [xo[s].key], [])
                    continue
                STT(xb[s].ap, xt[s].ap, rs[s].ap, wrep.ap, ALU.mult, ALU.mult, [xt[s].key, rs[s].key, wrep.key], [xb[s].key])
                for g in range(KC // 8):
                    pt = pb[bank % 4]
                    bank += 1
                    ptv = pt.ap.bitcast(BF16).rearrange("p (a b) -> p a b", a=8)
                    for j in range(8):
                        c = g * 8 + j
                        TR(ptv[:, j, :], xb[s].ap[:, c * 128:(c + 1) * 128], identb, [xb[s].key] + CKB, [pt.key])
                    CP(evac_eng(), xT[s].ap[:, g * 8:(g + 1) * 8, :], ptv, [pt.key], [xT[s].key])
                b_, sub = divmod(i, per)
                DMA("sync", dst_d[b_, :, :, sub * 128:(sub + 1) * 128], xT[s].ap, [xT[s].key], [dst_d.tensor.name])

        def phase_attn():
            A.reset()
            wA = [A.alloc("wA%d" % i, [KC, 384], BF16) for i in range(2)]
            xblk = [A.alloc("xblk%d" % i, [KC, 512], BF16) for i in range(2)]
            km = A.alloc("km", [NKP], F32)
            qT = A.alloc("qT", [T], BF16)
            kT = A.alloc("kT", [NAB * 512], BF16)
            vA = A.alloc("vA", [NKP, 128], BF16)
            bT = [A.alloc("bT%d" % i, [640], BF16) for i in range(2)]
            PT = A.alloc("PT", [NKP, 640], BF16)
            oT = [A.alloc("oT%d" % i, [T], BF16) for i in range(2)]
            rc = [A.alloc("rc%d" % i, [128], F32) for i in range(2)]
            ovs = [A.alloc("ov%d" % i, [128], F32) for i in range(2)]
            DMA("sync", km.ap, kmask, [], [km.key])
            xcnt = 0
            sc = 128 ** -0.5
            XD = [xnT_d.tensor.name]
            for h in range(HA):
                w = wA[h % 2]
                load_w(w, w_attn, h * 384, (h + 1) * 384, KC)
                b = bT[h % 2]
                DMA("gpsimd", b.ap, relbT[h], [], [b.key])
                for tbi in range(NAB):
                    tb = NWB - NAB + tbi
                    xb = xblk[xcnt % 2]
                    xcnt += 1
                    DMA("sync", xb.ap, xnT_d[tb], XD, [xb.key])
                    pk = pb[0]
                    for kc in range(KC):
                        MM(pk.ap, w.ap[:, kc, 128:256], xb.ap[:, kc, :], kc == 0, kc == KC - 1, [w.key, xb.key], [pk.key])
                    CP("scalar", kT.ap[:, tbi * 512:(tbi + 1) * 512], pk.ap, [pk.key], [kT.key])
                    if tbi >= 1:
                        pq = pb[1]
                        for kc in range(KC):
                            MM(pq.ap, w.ap[:, kc, 0:128], xb.ap[:, kc, :], kc == 0, kc == KC - 1, [w.key, xb.key], [pq.key])
                        TS(qT.ap[:, (tbi - 1) * 512:tbi * 512], pq.ap, sc, None, ALU.mult, None, [pq.key], [qT.key])
                    pvv = pb[2 + (tbi % 2)]
                    for j in range(4):
                        for kc in range(KC):
                            MM(pvv.ap[:, j * 128:(j + 1) * 128], xb.ap[:, kc, j * 128:(j + 1) * 128], w.ap[:, kc, 256:384],
                               kc == 0, kc == KC - 1, [w.key, xb.key], [pvv.key])
                    CP("vector", vA.ap[:, tbi * 4:(tbi + 1) * 4, :], pvv.ap.rearrange("p (a b) -> p a b", a=4), [pvv.key], [vA.key])
                import os as _os
                _cut = int(_os.environ.get('ATTN_CUT', '9'))
                if _cut < 2:
                    continue
                for kt in range(NKP):
                    q_lo = max(kt, 4)
                    q_hi = min(kt + 4, NKP - 1)
                    nq = (q_hi - q_lo + 1) * 128
                    jq0 = q_lo - kt
                    parts = [(0, min(nq, 512))]
                    if nq > 512:
                        parts.append((512, nq))
                    for pi, (c0, c1) in enumerate(parts):
                        ps = pb[4 + ((2 * kt + pi) % 4)]
                        n = c1 - c0
                        MM(ps.ap[:, 0:n], kT.ap[:, kt * 128:(kt + 1) * 128], qT.ap[:, (q_lo - 4) * 128 + c0:(q_lo - 4) * 128 + c1],
                           True, False, [kT.key, qT.key], [ps.key])
                        MM(ps.ap[:, 0:n], identb, b.ap[:, jq0 * 128 + c0:jq0 * 128 + c1], False, True, [b.key] + CKB, [ps.key])
                        ACT(PT.ap[:, kt, c0:c1], ps.ap[:, 0:n], AF.Exp, [ps.key, km.key], [PT.key + ":%d" % kt], bias=km.ap[:, kt:kt + 1])
                if _cut < 3:
                    continue
                o = oT[h % 2]
                for qo in range(NTT):
                    qt = qo + 4
                    po = pb[(qo % 2)]
                    kts = list(range(qt - 4, qt + 1))
                    for gi, (lhs, c0) in enumerate(((None, 0), (onesb, 128))):
                        for n_, kt in enumerate(kts):
                            off = (qt - max(kt, 4)) * 128
                            l = vA.ap[:, kt, :] if lhs is None else lhs
                            MM(po.ap[:, c0:c0 + 128], l, PT.ap[:, kt, off:off + 128], n_ == 0, n_ == len(kts) - 1,
                               [vA.key, PT.key + ":%d" % kt] + CKB, [po.key])
                    r = rc[qo % 2]
                    CP("scalar", r.ap, po.ap[:, 128:256], [po.key], [r.key])
                    RECIP(r.ap, r.ap, [r.key], [r.key])
                    ov = ovs[qo % 2]
                    CP("scalar", ov.ap, po.ap[:, 0:128], [po.key], [ov.key])
                    TT(o.ap[:, qo * 128:(qo + 1) * 128], ov.ap, r.ap, ALU.mult, [ov.key, r.key], [o.key])
                for tb in range(NTB):
                    DMA("sync", mixT_d[tb, :, h, :], o.ap[:, tb * 512:(tb + 1) * 512], [o.key], [mixT_d.tensor.name])

        def phase_ret():
            A.reset()
            W = A.alloc("wR", [KC, 768], BF16)
            xblk = [A.alloc("xblk%d" % i, [KC, 512], BF16) for i in range(2)]
            cs = [A.alloc("cs%d" % i, [512], F32) for i in range(2)]
            sn = [A.alloc("sn%d" % i, [512], F32) for i in range(2)]
            mt = A.alloc("mt", [HR, 128], F32)
            gt = A.alloc("gt", [HR, 128], F32)
            kd = A.alloc("kd", [HR, NWT], F32)
            gw = A.alloc("gw", [HR, 256], F32)
            krT = A.alloc("krT", [W2], BF16)
            qrT = A.alloc("qrT", [T], BF16)
            qtT = A.alloc("qtT", [T], BF16)
            ktm = A.alloc("ktm", [NWT, 128], BF16)
            vR = A.alloc("vR", [NWT, 256], BF16)
            sg = A.alloc("sg", [NTT, 256], BF16)
            raw = [A.alloc("raw%d" % i, [512], BF16) for i in range(2)]
            raw2 = [A.alloc("raw2%d" % i, [512], BF16) for i in range(2)]
            t1 = [A.alloc("t1%d" % i, [512], F32) for i in range(2)]
            t2 = [A.alloc("t2%d" % i, [512], F32) for i in range(2)]
            S = A.alloc("S", [256], F32)
            Sb = A.alloc("Sb", [256], BF16)
            AT = [A.alloc("AT%d" % i, [128], BF16) for i in range(2)]
            st6 = A.alloc("st6", [6], F32)
            mv = A.alloc("mv", [2], F32)
            rstd = A.alloc("rstd", [1], F32)
            yn = [A.alloc("yn%d" % i, [256], F32) for i in range(2)]
            rr = [A.alloc("rr%d" % i, [256], BF16) for i in range(2)]
            rT = A.alloc("rT", [2, T], BF16)
            tis = [A.alloc("ti%d" % i, [128], F32) for i in range(2)]
            kvs = A.alloc("kvs", [256], F32)
            ysbs = [A.alloc("ysb%d" % i, [256], F32) for i in range(2)]
            import os as _os
            _sub = _os.environ.get('RET_SUB', '')
            if 'd' not in _sub:
                DMA("sync", mt.ap, mtab.rearrange("p (h c) -> p h c", h=HR), [], [mt.key])
                DMA("sync", gt.ap, gtab.rearrange("p (h c) -> p h c", h=HR), [], [gt.key])
                DMA("sync", kd.ap, kdec.rearrange("p (h c) -> p h c", h=HR), [], [kd.key])
                DMA("sync", gw.ap, gnw.rearrange("p (h c) -> p h c", h=HR), [], [gw.key])
            XD = [xnT_d.tensor.name]
            xcnt = 0
            rcnt = 0

            def rotary(ps, dst_ap, dst_key, c, s_):
                nonlocal rcnt
                i = rcnt % 2
                rcnt += 1
                if 'r' in _sub:
                    CP("scalar", dst_ap, ps.ap, [ps.key], [dst_key])
                    return
                _rn = int(_os.environ.get('ROT_N', '9'))
                CP("scalar", raw[i].ap, ps.ap, [ps.key], [raw[i].key])
                psw = pb[6 + i]
                if _rn >= 2:
                    MM(psw.ap, permb, raw[i].ap, True, True, [raw[i].key] + CKB, [psw.key])
                if _rn >= 3:
                    TT(t1[i].ap, raw[i].ap, c.ap, ALU.mult, [raw[i].key, c.key], [t1[i].key])
                if _rn >= 4:
                    CP("scalar", raw2[i].ap, psw.ap, [psw.key], [raw2[i].key])
                    TT(t2[i].ap, raw2[i].ap, s_.ap, ALU.mult, [raw2[i].key, s_.key], [t2[i].key])
                if _rn >= 5:
                    TT(dst_ap, t1[i].ap, t2[i].ap, ALU.add, [t1[i].key, t2[i].key], [dst_key])

            import os as _os
            _sub = _os.environ.get('RET_SUB', '')
            for h in range(HR if 'x' not in _sub else 0):
                load_w(W, w_ret, h * 768, (h + 1) * 768, KC)
                if 'w' in _sub:
                    continue
                for tb in range(NWB):
                    own = tb >= NWB // 2
                    ob = tb - NWB // 2
                    xb = xblk[xcnt % 2]
                    c_ = cs[xcnt % 2]
                    s_ = sn[xcnt % 2]
                    xcnt += 1
                    DMA("sync", xb.ap, xnT_d[tb], XD, [xb.key])
                    DMA("sync", c_.ap, costab[:, tb * 512:(tb + 1) * 512], [], [c_.key])
                    DMA("sync", s_.ap, sintab[:, tb * 512:(tb + 1) * 512], [], [s_.key])
                    pk = pb[0]
                    for kc in range(KC):
                        MM(pk.ap, W.ap[:, kc, 128:256], xb.ap[:, kc, :], kc == 0, kc == KC - 1, [W.key, xb.key], [pk.key])
                    rotary(pk, krT.ap[:, tb * 512:(tb + 1) * 512], krT.key + ":%d" % tb, c_, s_)
                    if own and 'q' not in _sub:
                        pq = pb[1]
                        for kc in range(KC):
                            MM(pq.ap, W.ap[:, kc, 0:128], xb.ap[:, kc, :], kc == 0, kc == KC - 1, [W.key, xb.key], [pq.key])
                        rotary(pq, qrT.ap[:, ob * 512:(ob + 1) * 512], qrT.key + ":%d" % ob, c_, s_)
                        TT(qtT.ap[:, ob * 512:(ob + 1) * 512].rearrange("p (a b) -> p a b", a=4),
                           qrT.ap[:, ob * 512:(ob + 1) * 512].rearrange("p (a b) -> p a b", a=4),
                           gt.ap[:, h:h + 1, :].broadcast_to([128, 4, 128]), ALU.mult,
                           [qrT.key + ":%d" % ob, gt.key], [qtT.key + ":%d" % ob])
                    for j in range(4 if 'v' not in _sub else 0):
                        wt = tb * 4 + j
                        pvv = pb[2 + (j % 2)]
                        for kc in range(KC):
                            MM(pvv.ap[:, 0:256], xb.ap[:, kc, j * 128:(j + 1) * 128], W.ap[:, kc, 256:512], kc == 0, kc == KC - 1,
                               [W.key, xb.key], [pvv.key])
                        CP(evac_eng(), vR.ap[:, wt, :], pvv.ap[:, 0:256], [pvv.key], [vR.key + ":%d" % wt])
                        if own:
                            pg = pb[4 + (j % 2)]
                            for kc in range(KC):
                                MM(pg.ap[:, 0:256], xb.ap[:, kc, j * 128:(j + 1) * 128], W.ap[:, kc, 512:768], kc == 0, kc == KC - 1,
                                   [W.key, xb.key], [pg.key])
                            ACT(sg.ap[:, ob * 4 + j, :], pg.ap[:, 0:256], AF.Silu, [pg.key], [sg.key + ":%d" % (ob * 4 + j)])
                    ptk = pb[4 + (tb % 2)] if not own else pb[6 + (tb % 2)]
                    ptkv = ptk.ap.bitcast(BF16).rearrange("p (a b) -> p a b", a=8)
                    for j in range(4 if 't' not in _sub else 0):
                        wt = tb * 4 + j
                        TR(ptkv[:, j, :], krT.ap[:, wt * 128:(wt + 1) * 128], identb, [krT.key + ":%d" % tb] + CKB, [ptk.key])
                        TS(ktm.ap[:, wt, :], ptkv[:, j, :], kd.ap[:, h, wt:wt + 1], None, ALU.mult, None, [ptk.key, kd.key],
                           [ktm.key + ":%d" % wt])
                import os as _os
                _rc = int(_os.environ.get('RET_CUT', '9'))
                if _rc < 2:
                    continue
                pS = pb[0]
                npre = NWT // 2
                for wt in range(npre):
                    MM(pS.ap[:, 0:256], ktm.ap[:, wt, :], vR.ap[:, wt, :], wt == 0, wt == npre - 1,
                       [ktm.key + ":%d" % wt, vR.key + ":%d" % wt], [pS.key])
                CP("scalar", S.ap, pS.ap[:, 0:256], [pS.key], [S.key])
                CP("vector", Sb.ap, S.ap, [S.key], [Sb.key])
                g128 = gam[h] ** 128
                if _rc < 3:
                    continue
                for b_ in range(NTT):
                    wt = npre + b_
                    ob = b_ // 4
                    pi = pb[1 + (b_ % 2)]
                    MM(pi.ap[:, 0:128], krT.ap[:, wt * 128:(wt + 1) * 128], qrT.ap[:, b_ * 128:(b_ + 1) * 128], True, True,
                       [krT.key + ":%d" % (wt // 4), qrT.key + ":%d" % ob], [pi.key])
                    at = AT[b_ % 2]
                    ti = tis[b_ % 2]
                    CP("scalar", ti.ap, pi.ap[:, 0:128], [pi.key], [ti.key])
                    TT(at.ap, ti.ap, mt.ap[:, h, :], ALU.mult, [ti.key, mt.key], [at.key])
                    py = pb[3 + (b_ % 2)]
                    MM(py.ap[:, 0:256], at.ap, vR.ap[:, wt, :], True, False, [at.key, vR.key + ":%d" % wt], [py.key])
                    MM(py.ap[:, 0:256], qtT.ap[:, b_ * 128:(b_ + 1) * 128], Sb.ap, False, True, [qtT.key + ":%d" % ob, Sb.key], [py.key])
                    if b_ < NTT - 1:
                        pkv = pb[5]
                        MM(pkv.ap[:, 0:256], ktm.ap[:, wt, :], vR.ap[:, wt, :], True, True,
                           [ktm.key + ":%d" % wt, vR.key + ":%d" % wt], [pkv.key])
                        CP("scalar", kvs.ap, pkv.ap[:, 0:256], [pkv.key], [kvs.key])
                        STT(S.ap, S.ap, g128, kvs.ap, ALU.mult, ALU.add, [S.key, kvs.key], [S.key])
                        CP("scalar", Sb.ap, S.ap, [S.key], [Sb.key])
                    if _rc < 4:
                        continue
                    ysb = ysbs[b_ % 2]
                    CP("scalar", ysb.ap, py.ap[:, 0:256], [py.key], [ysb.key])
                    P.add("vector", lambda e, o_=st6.ap, i_=ysb.ap: e.bn_stats(out=o_, in_=i_), [ysb.key], [st6.key])
                    P.add("vector", lambda e, o_=mv.ap, i_=st6.ap: e.bn_aggr(out=o_, in_=i_), [st6.key], [mv.key])
                    TS(rstd.ap, mv.ap[:, 1:2], EPS, None, ALU.add, None, [mv.key], [rstd.key])
                    ACT(rstd.ap, rstd.ap, AF.Sqrt, [rstd.key], [rstd.key])
                    RECIP(rstd.ap, rstd.ap, [rstd.key], [rstd.key])
                    y_ = yn[b_ % 2]
                    TS(y_.ap, ysb.ap, mv.ap[:, 0:1], rstd.ap, ALU.subtract, ALU.mult, [ysb.key, mv.key, rstd.key], [y_.key])
                    TT(y_.ap, y_.ap, gw.ap[:, h, :], ALU.mult, [y_.key, gw.key], [y_.key])
                    r_ = rr[b_ % 2]
                    TT(r_.ap, y_.ap, sg.ap[:, b_, :], ALU.mult, [y_.key, sg.key + ":%d" % b_], [r_.key])
                    ptr = pb[6 + (b_ % 2)]
                    ptrv = ptr.ap.bitcast(BF16).rearrange("p (a b) -> p a b", a=8)
                    for jj in range(2):
                        TR(ptrv[:, jj, :], r_.ap[:, jj * 128:(jj + 1) * 128], identb, [r_.key] + CKB, [ptr.key])
                    CP("scalar", rT.ap[:, :, b_ * 128:(b_ + 1) * 128], ptrv[:, 0:2, :], [ptr.key], [rT.key])
                for jj in range(2):
                    for tb in range(NTB):
                        DMA("sync", mixT_d[tb, :, HA + 2 * h + jj, :], rT.ap[:, jj, tb * 512:(tb + 1) * 512], [rT.key], [mixT_d.tensor.name])

        def phase_linear_A(actT_d, w_ap, Dout, res_d, dst_d):
            A.reset()
            act = [A.alloc("act%d" % i, [KC, 512], BF16) for i in range(NTB)]
            wb = [A.alloc("wb%d" % i, [KC, 512], BF16) for i in range(2)]
            rt = [A.alloc("rt%d" % i, [512], F32) for i in range(3)]
            ot = [A.alloc("ot%d" % i, [512], F32) for i in range(3)]
            for tb in range(NTB):
                DMA("sync", act[tb].ap, actT_d[tb], [actT_d.tensor.name], [act[tb].key])
            ncb = Dout // 512
            load_w(wb[0], w_ap, 0, 512, KC)
            cnt = 0
            for cb in range(ncb):
                w = wb[cb % 2]
                if cb + 1 < ncb:
                    load_w(wb[(cb + 1) % 2], w_ap, (cb + 1) * 512, (cb + 2) * 512, KC)
                for tt in range(NTT):
                    s = cnt % 3
                    pbk = pb[cnt % 4]
                    cnt += 1
                    DMA("sync", rt[s].ap, res_d[tt * 128:(tt + 1) * 128, cb * 512:(cb + 1) * 512], [res_d.tensor.name], [rt[s].key])
                    a = act[tt // 4]
                    for kc in range(KC):
                        MM(pbk.ap, a.ap[:, kc, (tt % 4) * 128:(tt % 4 + 1) * 128], w.ap[:, kc, :], kc == 0, kc == KC - 1,
                           [a.key, w.key], [pbk.key])
                    CP("scalar", ot[s].ap, pbk.ap, [pbk.key], [ot[s].key])
                    TT(ot[s].ap, ot[s].ap, rt[s].ap, ALU.add, [ot[s].key, rt[s].key], [ot[s].key])
                    DMA("sync", dst_d[tt * 128:(tt + 1) * 128, cb * 512:(cb + 1) * 512], ot[s].ap, [ot[s].key], [dst_d.tensor.name])

        def phase_xattn():
            A.reset()
            hn = [A.alloc("hn%d" % i, [KC, 512], BF16) for i in range(NTB)]
            wb = [A.alloc("wb%d" % i, [KC, 512], BF16) for i in range(2)]
            kTx = A.alloc("kTx", [KC, MEM], BF16)
            vX = A.alloc("vX", [2, D], BF16)
            mark = A.off
            mT = A.alloc("mT", [KC, MEM], BF16)
            DMA("sync", mT.ap, memT_d[0], [memT_d.tensor.name], [mT.key])
            for tb in range(NTB):
                DMA("sync", hn[tb].ap, hn2T_d[tb], [hn2T_d.tensor.name], [hn[tb].key])
            wcnt = 0
            bcnt = 0
            for cb in range(D // 512):
                w = wb[wcnt % 2]
                wcnt += 1
                load_w(w, wkv, cb * 512, (cb + 1) * 512, KC)
                for c4 in range(4):
                    pk = pb[bcnt % 4]
                    bcnt += 1
                    for kc in range(KC):
                        MM(pk.ap[:, 0:MEM], w.ap[:, kc, c4 * 128:(c4 + 1) * 128], mT.ap[:, kc, :], kc == 0, kc == KC - 1,
                           [w.key, mT.key], [pk.key])
                    CP(evac_eng(), kTx.ap[:, cb * 4 + c4, :], pk.ap[:, 0:MEM], [pk.key], [kTx.key])
            for cb in range(D // 512):
                w = wb[wcnt % 2]
                wcnt += 1
                load_w(w, wkv, D + cb * 512, D + (cb + 1) * 512, KC)
                for mtile in range(2):
                    pvv = pb[bcnt % 4]
                    bcnt += 1
                    for kc in range(KC):
                        MM(pvv.ap, mT.ap[:, kc, mtile * 128:(mtile + 1) * 128], w.ap[:, kc, :], kc == 0, kc == KC - 1,
                           [w.key, mT.key], [pvv.key])
                    CP(evac_eng(), vX.ap[:, mtile, cb * 512:(cb + 1) * 512], pvv.ap, [pvv.key], [vX.key])
            P.barrier()
            A.off = mark
            A.gen += 1
            qTx = A.alloc("qTx", [CX, T], BF16)
            PTx = [A.alloc("PTx%d" % i, [512], BF16) for i in range(2)]
            rcx = A.alloc("rcx", [512], F32)
            oTx = [A.alloc("oTx%d" % i, [512], BF16) for i in range(3)]
            otmp = A.alloc("otmp", [512], F32)
            sc = DX ** -0.5
            ocnt = 0
            for hx in range(XH):
                for cb2 in range(DX // 512):
                    w = wb[wcnt % 2]
                    wcnt += 1
                    c0 = hx * DX + cb2 * 512
                    load_w(w, wq, c0, c0 + 512, KC)
                    for tb in range(NTB):
                        for c4 in range(4):
                            pq = pb[bcnt % 4]
                            bcnt += 1
                            for kc in range(KC):
                                MM(pq.ap, w.ap[:, kc, c4 * 128:(c4 + 1) * 128], hn[tb].ap[:, kc, :], kc == 0, kc == KC - 1,
                                   [w.key, hn[tb].key], [pq.key])
                            TS(qTx.ap[:, cb2 * 4 + c4, tb * 512:(tb + 1) * 512], pq.ap, sc, None, ALU.mult, None, [pq.key], [qTx.key])
                for tb in range(NTB):
                    for kt in range(2):
                        ps = pb[4 + kt]
                        for c in range(CX):
                            MM(ps.ap, kTx.ap[:, hx * CX + c, kt * 128:(kt + 1) * 128], qTx.ap[:, c, tb * 512:(tb + 1) * 512],
                               c == 0, c == CX - 1, [kTx.key, qTx.key], [ps.key])
                        ACT(PTx[kt].ap, ps.ap, AF.Exp, [ps.key], [PTx[kt].key])
                    pd = pb[6]
                    for kt in range(2):
                        MM(pd.ap, onesb, PTx[kt].ap, kt == 0, kt == 1, [PTx[kt].key] + CKB, [pd.key])
                    CP("scalar", rcx.ap, pd.ap, [pd.key], [rcx.key])
                    RECIP(rcx.ap, rcx.ap, [rcx.key], [rcx.key])
                    for c in range(CX):
                        po = pb[bcnt % 4]
                        bcnt += 1
                        fc = hx * CX + c
                        for kt in range(2):
                            MM(po.ap, vX.ap[:, kt, fc * 128:(fc + 1) * 128], PTx[kt].ap, kt == 0, kt == 1,
                               [vX.key, PTx[kt].key], [po.key])
                        o = oTx[ocnt % 3]
                        ocnt += 1
                        CP("scalar", otmp.ap, po.ap, [po.key], [otmp.key])
                        TT(o.ap, otmp.ap, rcx.ap, ALU.mult, [otmp.key, rcx.key], [o.key])
                        DMA("sync", oT_d[tb, :, fc, :], o.ap, [o.key], [oT_d.tensor.name])

        def phase_peer_qr():
            A.reset()
            qpT = A.alloc("qpT", [16, T], BF16)
            sub = A.alloc("sub", [16, 128], BF16)
            mark = A.off
            hn = [A.alloc("hn%d" % i, [KC, 512], BF16) for i in range(NTB)]
            wb = [A.alloc("wb%d" % i, [KC, 512], BF16) for i in range(2)]
            DMA("gpsimd", sub.ap, subT.rearrange("p (a b) -> p a b", a=16), [], [sub.key])
            for tb in range(NTB):
                DMA("sync", hn[tb].ap, hn3T_d[tb], [hn3T_d.tensor.name], [hn[tb].key])
            bcnt = 0
            for cb in range(4):
                w = wb[cb % 2]
                load_w(w, pwq, cb * 512, (cb + 1) * 512, KC)
                for tb in range(NTB):
                    for c4 in range(4):
                        pq = pb[bcnt % 4]
                        bcnt += 1
                        for kc in range(KC):
                            MM(pq.ap, w.ap[:, kc, c4 * 128:(c4 + 1) * 128], hn[tb].ap[:, kc, :], kc == 0, kc == KC - 1,
                               [w.key, hn[tb].key], [pq.key])
                        CP(evac_eng(), qpT.ap[:, cb * 4 + c4, tb * 512:(tb + 1) * 512], pq.ap, [pq.key], [qpT.key])
            P.barrier()
            A.off = mark
            A.gen += 1
            qk = [qpT.key]
            s = A.alloc("s", [16, 128], F32)
            s2 = A.alloc("s2", [16, 128], F32)
            T16 = A.alloc("T16", [16, 16], F32)
            I16 = A.alloc("I16", [16, 16], U32)
            I16f = A.alloc("I16f", [16, 16], F32)
            cand = A.alloc("cand", [8, 256], F32)
            cand2 = A.alloc("cand2", [8, 256], F32)
            B16 = A.alloc("B16", [8, 16], F32)
            C16 = A.alloc("C16", [8, 16], U32)
            Ai = A.alloc("Ai", [8, 16], U32)
            Bi = A.alloc("Bi", [8, 16], U32)
            Af = A.alloc("Af", [8, 16], F32)
            Bf = A.alloc("Bf", [8, 16], F32)
            eb = A.alloc("eb", [8, 16], F32)
            Z = A.alloc("Z", [8], F32)
            eq = A.alloc("eq", [128, 16], F32)
            R = A.alloc("R", [3, 128], F32)
            RT = A.alloc("RT", [3, 128], F32)
            OI = A.alloc("OI", [128, 128], BF16)
            OJ = A.alloc("OJ", [128, 128], BF16)
            Gt = A.alloc("Gt", [128, 128], BF16)
            subv = sub.ap
            K = lambda *a: list(a)
            for tt in range(NTT):
                for hp in range(16):
                    bk = pb[hp // 4]
                    MM(bk.ap[:, (hp % 4) * 128:(hp % 4 + 1) * 128], qpT.ap[:, hp, tt * 128:(tt + 1) * 128], subv[:, hp, :], True, True,
                       qk + [sub.key], [bk.key])
                for q in range(4):
                    CP(evac_eng(), s.ap[:, q * 4:(q + 1) * 4, :], pb[q].ap.rearrange("p (a b) -> p a b", a=4), [pb[q].key], [s.key])
                for hp in range(16):
                    sv = s.ap[:, hp, :]
                    s2v = s2.ap[:, hp, :]
                    P.add("vector", lambda e, o=T16.ap[:, hp, 0:8], i=sv: e.max(out=o, in_=i), [s.key], [T16.key])
                    P.add("vector", lambda e, o=I16.ap[:, hp, 0:8], m=T16.ap[:, hp, 0:8], i=sv: e.max_index(out=o, in_max=m, in_values=i),
                          [s.key, T16.key], [I16.key])
                    P.add("vector", lambda e, o=s2v, m=T16.ap[:, hp, 0:8], i=sv: e.match_replace(out=o, in_to_replace=m, in_values=i, imm_value=NEG),
                          [s.key, T16.key], [s2.key])
                    P.add("vector", lambda e, o=T16.ap[:, hp, 8:16], i=s2v: e.max(out=o, in_=i), [s2.key], [T16.key])
                    P.add("vector", lambda e, o=I16.ap[:, hp, 8:16], m=T16.ap[:, hp, 8:16], i=s2v: e.max_index(out=o, in_max=m, in_values=i),
                          [s2.key, T16.key], [I16.key])
                CP("vector", I16f.ap, I16.ap, [I16.key], [I16f.key])
                T16v = T16.ap.rearrange("p (h two) k -> p h two k", two=2)
                I16v = I16f.ap.rearrange("p (h two) k -> p h two k", two=2)
                candv = cand.ap.rearrange("p h (a b) -> p h a b", a=16)
                TT(candv, T16v[:, :, 0, :].unsqueeze(3).broadcast_to([128, 8, 16, 16]),
                   T16v[:, :, 1, :].unsqueeze(2).broadcast_to([128, 8, 16, 16]), ALU.add, [T16.key], [cand.key])
                for h in range(8):
                    cv = cand.ap[:, h, :]
                    c2v = cand2.ap[:, h, :]
                    P.add("vector", lambda e, o=B16.ap[:, h, 0:8], i=cv: e.max(out=o, in_=i), [cand.key], [B16.key])
                    P.add("vector", lambda e, o=C16.ap[:, h, 0:8], m=B16.ap[:, h, 0:8], i=cv: e.max_index(out=o, in_max=m, in_values=i),
                          [cand.key, B16.key], [C16.key])
                    P.add("vector", lambda e, o=c2v, m=B16.ap[:, h, 0:8], i=cv: e.match_replace(out=o, in_to_replace=m, in_values=i, imm_value=NEG),
                          [cand.key, B16.key], [cand2.key])
                    P.add("vector", lambda e, o=B16.ap[:, h, 8:16], i=c2v: e.max(out=o, in_=i), [cand2.key], [B16.key])
                    P.add("vector", lambda e, o=C16.ap[:, h, 8:16], m=B16.ap[:, h, 8:16], i=c2v: e.max_index(out=o, in_max=m, in_values=i),
                          [cand2.key, B16.key], [C16.key])
                TT(eb.ap, B16.ap, B16.ap[:, :, 0:1].broadcast_to([128, 8, 16]), ALU.subtract, [B16.key], [eb.key])
                ACT(eb.ap, eb.ap, AF.Exp, [eb.key], [eb.key])
                P.add("vector", lambda e, o=Z.ap, i=eb.ap: e.tensor_reduce(out=o, in_=i, axis=AX.X, op=ALU.add), [eb.key], [Z.key])
                RECIP(Z.ap, Z.ap, [Z.key], [Z.key])
                TT(R.ap[:, 2, :].rearrange("p (h k) -> p h k", h=8), eb.ap, Z.ap.unsqueeze(2).broadcast_to([128, 8, 16]), ALU.mult,
                   [eb.key, Z.key], [R.key + ":2"])
                P.add("vector", lambda e, o=Ai.ap, i=C16.ap: e.tensor_single_scalar(out=o, in_=i, scalar=4, op=ALU.logical_shift_right),
                      [C16.key], [Ai.key])
                P.add("vector", lambda e, o=Bi.ap, i=C16.ap: e.tensor_single_scalar(out=o, in_=i, scalar=15, op=ALU.bitwise_and),
                      [C16.key], [Bi.key])
                CP("vector", Af.ap, Ai.ap, [Ai.key], [Af.key])
                CP("vector", Bf.ap, Bi.ap, [Bi.key], [Bf.key])
                for r_i, (sel, half) in enumerate(((Af, 0), (Bf, 1))):
                    TT(eq.ap, iotaf[:, 0:16].unsqueeze(1).broadcast_to([128, 128, 16]),
                       sel.ap.rearrange("p h k -> p (h k)").unsqueeze(2).broadcast_to([128, 128, 16]), ALU.is_equal,
                       [sel.key] + CK, [eq.key])
                    eq4 = eq.ap.rearrange("p (h k) a -> p h k a", h=8)
                    TT(eq4, eq4, I16v[:, :, half, :].unsqueeze(2).broadcast_to([128, 8, 16, 16]), ALU.mult, [eq.key, I16f.key], [eq.key])
                    P.add("vector", lambda e, o=R.ap[:, r_i, :], i=eq.ap: e.tensor_reduce(out=o, in_=i, axis=AX.X, op=ALU.add),
                          [eq.key], [R.key + ":%d" % r_i])
                ptR = pb[4]
                for r_i in range(3):
                    TR(ptR.ap[:, r_i * 128:(r_i + 1) * 128], R.ap[:, r_i, :], identf, [R.key + ":%d" % r_i] + CK, [ptR.key])
                CP("scalar", RT.ap, ptR.ap[:, 0:384].rearrange("p (a b) -> p a b", a=3), [ptR.key], [RT.key])
                TT(OI.ap, iotaf.unsqueeze(1).broadcast_to([128, 128, 128]), RT.ap[:, 0, :].unsqueeze(2).broadcast_to([128, 128, 128]),
                   ALU.is_equal, [RT.key] + CK, [OI.key])
                TT(OJ.ap, iotaf.unsqueeze(1).broadcast_to([128, 128, 128]), RT.ap[:, 1, :].unsqueeze(2).broadcast_to([128, 128, 128]),
                   ALU.is_equal, [RT.key] + CK, [OJ.key])
                TT(OJ.ap, OJ.ap, RT.ap[:, 2, :].unsqueeze(2).broadcast_to([128, 128, 128]), ALU.mult, [OJ.key, RT.key], [OJ.key])
                for t4 in range(32):
                    pg = pb[5 + (t4 % 3)]
                    for u in range(4):
                        t = t4 * 4 + u
                        MM(pg.ap[:, u * 128:(u + 1) * 128], OJ.ap[:, t, :], OI.ap[:, t, :], True, True, [OJ.key, OI.key], [pg.key])
                    CP(evac_eng(), Gt.ap[:, :, t4 * 4:(t4 + 1) * 4], pg.ap.rearrange("p (t i) -> p i t", t=4), [pg.key], [Gt.key])
                for i0 in range(0, 128, 16):
                    DMA("sync", G_d[i0:i0 + 16, :, tt * 128:(tt + 1) * 128].rearrange("i j t -> j i t"), Gt.ap[:, i0:i0 + 16, :],
                        [Gt.key], [G_d.tensor.name])

        def phase_peer_u():
            A.reset()
            hn = [A.alloc("hn%d" % i, [KC, 512], BF16) for i in range(NTB)]
            wb = [A.alloc("wb%d" % i, [KC, 512], BF16) for i in range(2)]
            Gi = [A.alloc("Gi%d" % i, [T], BF16) for i in range(3)]
            Aa = [A.alloc("Aa%d" % i, [512], BF16) for i in range(2)]
            GA = [A.alloc("GA%d" % i, [T], BF16) for i in range(3)]
            for tb in range(NTB):
                DMA("sync", hn[tb].ap, hn3T_d[tb], [hn3T_d.tensor.name], [hn[tb].key])
            load_w(wb[0], uT, 0, 512, KC)
            bcnt = 0
            for ib4 in range(NE // 4):
                w = wb[ib4 % 2]
                if ib4 + 1 < NE // 4:
                    load_w(wb[(ib4 + 1) % 2], uT, (ib4 + 1) * 512, (ib4 + 2) * 512, KC)
                for e4 in range(4):
                    i = ib4 * 4 + e4
                    g = Gi[i % 3]
                    ga = GA[i % 3]
                    DMA("sync", g.ap, G_d[i], [G_d.tensor.name], [g.key])
                    for tb in range(NTB):
                        pa = pb[bcnt % 6]
                        a_ = Aa[bcnt % 2]
                        bcnt += 1
                        for kc in range(KC):
                            MM(pa.ap, w.ap[:, kc, e4 * 128:(e4 + 1) * 128], hn[tb].ap[:, kc, :], kc == 0, kc == KC - 1,
                               [w.key, hn[tb].key], [pa.key])
                        ACT(a_.ap, pa.ap, AF.Gelu, [pa.key], [a_.key])
                        TT(ga.ap[:, tb * 512:(tb + 1) * 512], a_.ap, g.ap[:, tb * 512:(tb + 1) * 512], ALU.mult, [a_.key, g.key], [ga.key])
                    DMA("sync", GA_d[i], ga.ap, [ga.key], [GA_d.tensor.name])

        def phase_peer_v():
            A.reset()
            EG = 8
            vb = [A.alloc("vb%d" % i, [EG, 512], BF16) for i in range(2)]
            gg = [A.alloc("gg%d" % i, [EG, T], BF16) for i in range(2)]
            rt = [A.alloc("rt%d" % i, [512], F32) for i in range(2)]
            ot = [A.alloc("ot%d" % i, [512], F32) for i in range(2)]
            ng = NE // EG
            cnt = 0
            for cb in range(D // 512):
                for ig in range(ng):
                    v_ = vb[cnt % 2]
                    g_ = gg[cnt % 2]
                    cnt += 1
                    DMA("gpsimd", v_.ap, pv[ig * EG * 128:(ig + 1) * EG * 128, cb * 512:(cb + 1) * 512].rearrange("(e p) n -> p e n", p=128),
                        [], [v_.key])
                    DMA("sync", g_.ap, GA_d[ig * EG:(ig + 1) * EG].rearrange("e p t -> p e t"), [GA_d.tensor.name], [g_.key])
                    for e_ in range(EG):
                        first = (ig == 0 and e_ == 0)
                        last = (ig == ng - 1 and e_ == EG - 1)
                        for tt in range(NTT):
                            MM(pb[tt].ap, g_.ap[:, e_, tt * 128:(tt + 1) * 128], v_.ap[:, e_, :], first, last, [g_.key, v_.key], [pb[tt].key])
                for tt in range(NTT):
                    s = tt % 2
                    DMA("sync", rt[s].ap, h2_d[tt * 128:(tt + 1) * 128, cb * 512:(cb + 1) * 512], [h2_d.tensor.name], [rt[s].key])
                    CP("scalar", ot[s].ap, pb[tt].ap, [pb[tt].key], [ot[s].key])
                    TT(ot[s].ap, ot[s].ap, rt[s].ap, ALU.add, [ot[s].key, rt[s].key], [ot[s].key])
                    DMA("sync", h3_d[tt * 128:(tt + 1) * 128, cb * 512:(cb + 1) * 512], ot[s].ap, [ot[s].key], [h3_d.tensor.name])

        sched = [
            lambda: phase_nt(xw, W2, n1w, xnT_d, 512),
            phase_attn,
            phase_ret,
            lambda: phase_linear_A(mixT_d, w_out, D, xw[T:W2, :], h1_d),
            lambda: phase_nt(h1_d, T, n2w, hn2T_d, 512),
            lambda: phase_nt(memb, MEM, nmw, memT_d, MEM),
            phase_xattn,
            lambda: phase_linear_A(oT_d, wo, D, h1_d, h2_d),
            lambda: phase_nt(h2_d, T, n3w, hn3T_d, 512),
            phase_peer_qr,
            phase_peer_u,
            phase_peer_v,
            lambda: phase_nt(h3_d, T, nfw, None, 512, final_out=out),
        ]
        for i, ph in enumerate(sched):
            if nph is not None and i >= nph:
                break
            if i:
                P.barrier()
            ph()
        P.emit()
    return nc


def _host_tables(D, T, half):
    W2 = 2 * T
    NWT = W2 // 128
    HR = D // 512
    pos = np.arange(-T, T, dtype=np.float64) + half * T
    inv_freq = 1.0 / (10000.0 ** (np.arange(0, 128, 2, dtype=np.float64) / 128.0))
    ang = pos[None, :] * inv_freq[:, None]
    cos = np.cos(ang)
    sin = np.sin(ang)
    costab = np.concatenate([cos, cos], 0).astype(np.float32)
    sintab = np.concatenate([-sin, sin], 0).astype(np.float32)
    lg = np.log1p(-np.power(2.0, -5.0 - np.arange(HR, dtype=np.float64)))
    p = np.arange(128, dtype=np.float64)
    mk = p[:, None]
    cq = p[None, :]
    valid = (mk // 64) <= (cq // 64)
    mtab = np.zeros((128, HR, 128), np.float64)
    gtab = np.zeros((128, HR, 128), np.float64)
    kdec = np.zeros((128, HR, NWT), np.float64)
    for h in range(HR):
        mtab[:, h, :] = np.where(valid, np.exp(lg[h] * np.abs(cq - mk)), 0.0) * (128 ** -0.5)
        gtab[:, h, :] = np.exp(lg[h] * (cq + 1.0))
        for wt in range(NWT):
            if wt < NWT // 2:
                t = wt * 128 + p
                kdec[:, h, wt] = np.exp(lg[h] * (T - 1 - t)) * (128 ** -0.5)
            else:
                kdec[:, h, wt] = np.exp(lg[h] * (127 - p)) * (128 ** -0.5)
    return (costab, sintab, mtab.reshape(128, -1).astype(np.float32), gtab.reshape(128, -1).astype(np.float32),
            kdec.reshape(128, -1).astype(np.float32))


def _rel_bias_table(rb):
    mk = np.arange(128)[:, None]
    col = np.arange(640)[None, :]
    jq = col // 128
    cq = col % 128
    dist = 128 * jq + cq - mk
    idx = np.clip(dist, -256, 256) + 256
    diff = 2 * jq + (cq // 64) - (mk // 64)
    valid = (diff >= 0) & (diff <= 8)
    tab = rb[:, idx]
    return np.where(valid[None], tab, np.float32(NEG)).astype(np.float32)


_CACHE = {}


def _get_prog(D, T):
    key = (D, T)
    if key not in _CACHE:
        _CACHE[key] = build_program(D, T)
    return _CACHE[key]


def kernel(x, mem, norm1_w, w_in, attn_rel_bias, ret_gn_w, w_out, norm2_w, mem_norm_w,
           xattn_wq, xattn_wkv, xattn_wo, norm3_w, peer_wq, peer_subkeys, peer_u, peer_v,
           final_norm_w):
    f = lambda a: np.ascontiguousarray(np.asarray(a, dtype=np.float32))
    x = f(x)
    mem = f(mem)
    B, S, D = x.shape
    T = S // 2
    HA = D // 256
    HR = D // 512
    AW = HA * 128
    win = f(w_in)[0]
    aq, ak, av = win[:, 0:AW], win[:, AW:2 * AW], win[:, 2 * AW:3 * AW]
    o = 3 * AW
    rq, rk = win[:, o:o + HR * 128], win[:, o + HR * 128:o + 2 * HR * 128]
    o2 = o + 2 * HR * 128
    rv, rg = win[:, o2:o2 + HR * 256], win[:, o2 + HR * 256:o2 + 2 * HR * 256]
    w_attn = np.concatenate([np.concatenate([aq[:, h * 128:(h + 1) * 128], ak[:, h * 128:(h + 1) * 128], av[:, h * 128:(h + 1) * 128]], 1)
                             for h in range(HA)], 1)
    w_ret = np.concatenate([np.concatenate([rq[:, h * 128:(h + 1) * 128], rk[:, h * 128:(h + 1) * 128],
                                            rv[:, h * 256:(h + 1) * 256], rg[:, h * 256:(h + 1) * 256]], 1) for h in range(HR)], 1)
    rep = lambda v: np.ascontiguousarray(np.broadcast_to(f(v).reshape(1, -1), (128, f(v).size)))
    relbT = _rel_bias_table(f(attn_rel_bias)[0])
    sub = f(peer_subkeys)[0]
    subT = np.ascontiguousarray(sub.transpose(3, 0, 1, 2).reshape(128, 16 * 128))
    uT = np.ascontiguousarray(f(peer_u)[0].T)
    consts = np.zeros((128, 512), np.float32)
    consts[:, 0:128] = np.eye(128, dtype=np.float32)
    consts[:, 128:256] = np.roll(np.eye(128, dtype=np.float32), 64, axis=0)
    consts[:, 256:384] = 1.0
    consts[:, 384:512] = np.arange(128, dtype=np.float32)[None, :]
    shared = dict(
        n1w=rep(norm1_w), n2w=rep(norm2_w), nmw=rep(mem_norm_w), n3w=rep(norm3_w), nfw=rep(final_norm_w),
        w_attn=np.ascontiguousarray(w_attn), w_ret=np.ascontiguousarray(w_ret), relbT=relbT,
        gnw=rep(ret_gn_w), w_out=f(w_out)[0], wq=f(xattn_wq)[0], wkv=f(xattn_wkv)[0], wo=f(xattn_wo)[0],
        pwq=f(peer_wq)[0], subT=subT, uT=uT, pv=f(peer_v)[0], consts=consts,
    )
    NKP = (1 + T // 512) * 4
    in_maps = []
    for b in range(B):
        for half in range(2):
            xw = np.zeros((2 * T, D), np.float32)
            if half == 1:
                xw[:T] = x[b, :T]
            xw[T:] = x[b, half * T:(half + 1) * T]
            km = np.zeros((128, NKP), np.float32)
            if half == 0:
                km[:, 0:4] = NEG
            costab, sintab, mtab, gtab, kdec = _host_tables(D, T, half)
            m = dict(shared)
            m.update(xw=xw, memb=mem[b], kmask=km, costab=costab, sintab=sintab, mtab=mtab, gtab=gtab, kdec=kdec)
            in_maps.append(m)
    nc = _get_prog(D, T)
    res = run_bass_kernel_spmd(nc, in_maps, core_ids=list(range(len(in_maps))))
    outp = np.zeros((B, S, D), np.float32)
    for b in range(B):
        for half in range(2):
            outp[b, half * T:(half + 1) * T] = res.results[b * 2 + half]["out"]
    return outp
```

```python
import contextlib
import math
import numpy as np
import concourse.bass as bass
import concourse.mybir as mybir
from concourse.bass_utils import run_bass_kernel_spmd

F32 = mybir.dt.float32
BF16 = mybir.dt.bfloat16
U32 = mybir.dt.uint32
AF = mybir.ActivationFunctionType
ALU = mybir.AluOpType
AX = mybir.AxisListType

EPS = 1e-6
NEG = -1e30
ENGS = ("tensor", "vector", "scalar", "gpsimd", "sync")


class Op:
    __slots__ = ("eng", "fn", "waits", "signal", "seq", "is_dma", "dsem", "dval", "pos", "ewaits")

    def __init__(self, eng, fn, is_dma):
        self.eng = eng
        self.fn = fn
        self.is_dma = is_dma
        self.waits = []
        self.signal = False
        self.seq = None
        self.dsem = None
        self.dval = None


class Prog:
    def __init__(self, nc, n_dma_sems=24):
        self.nc = nc
        self.ops = {e: [] for e in ENGS}
        self.last_w = {}
        self.readers = {}
        self.n_dma_sems = n_dma_sems
        self.dma_rr = {e: 0 for e in ENGS}
        self.dma_last = {}
        self.dma_cnt = {}
        self.pending_barrier = {e: None for e in ENGS}

    def barrier(self):
        b = []
        for e in ENGS:
            for op in reversed(self.ops[e]):
                if not op.is_dma:
                    b.append(op)
                    break
        b.extend(self.dma_last.values())
        for e in ENGS:
            self.pending_barrier[e] = b
        self.last_w = {}
        self.readers = {}

    def add(self, eng, fn, reads=(), writes=(), dma=False):
        op = Op(eng, fn, dma)
        w = op.waits
        pb = self.pending_barrier[eng]
        if pb is not None:
            w.extend(p for p in pb if p is not op)
            self.pending_barrier[eng] = None
        for k in reads:
            p = self.last_w.get(k)
            if p is not None:
                w.append(p)
        for k in writes:
            p = self.last_w.get(k)
            if p is not None:
                w.append(p)
            r = self.readers.get(k)
            if r:
                w.extend(r)
        for k in reads:
            self.readers.setdefault(k, []).append(op)
        for k in writes:
            self.last_w[k] = op
            self.readers[k] = []
        if eng == "tensor":
            op.waits = w = [p for p in w if p.eng != "tensor" or p.is_dma]
        if dma:
            slot = self.dma_rr[eng]
            self.dma_rr[eng] = (slot + 1) % self.n_dma_sems
            prev = self.dma_last.get((eng, slot))
            if prev is not None:
                w.append(prev)
            self.dma_last[(eng, slot)] = op
            c = self.dma_cnt.get((eng, slot), 0) + 1
            self.dma_cnt[(eng, slot)] = c
            op.dsem = (eng, slot)
            op.dval = 16 * c
        self.ops[eng].append(op)
        return op

    def emit(self):
        nc = self.nc
        for e in ENGS:
            for i, op in enumerate(self.ops[e]):
                op.pos = i
        for e in ENGS:
            for op in self.ops[e]:
                best = {}
                for p in op.waits:
                    if not p.is_dma:
                        b = best.get(p.eng)
                        if b is None or p.pos > b.pos:
                            best[p.eng] = p
                op.ewaits = list(best.values())
                for p in op.ewaits:
                    p.signal = True
        for e in ENGS:
            n = 0
            for op in self.ops[e]:
                if op.signal and not op.is_dma:
                    n += 1
                    op.seq = n
        with contextlib.ExitStack() as st:
            esem = {e: st.enter_context(nc.semaphore("es_" + e)) for e in ENGS}
            dsem = {}
            for e in ENGS:
                if any(o.is_dma for o in self.ops[e]):
                    for s in range(self.n_dma_sems):
                        dsem[(e, s)] = st.enter_context(nc.semaphore("ds_%s_%d" % (e, s)))
            block = st.enter_context(nc.Block())

            def run(eng_name):
                def body(eng):
                    known_e = {}
                    known_d = {}
                    for op in self.ops[eng_name]:
                        need_e = {}
                        need_d = {}
                        for p in op.waits:
                            if p.is_dma:
                                if need_d.get(p.dsem, 0) < p.dval:
                                    need_d[p.dsem] = p.dval
                        for p in op.ewaits:
                            need_e[p.eng] = p.seq
                        for k, v in need_e.items():
                            if known_e.get(k, 0) < v:
                                eng.wait_ge(esem[k], v)
                                known_e[k] = v
                        for k, v in need_d.items():
                            if known_d.get(k, 0) < v:
                                eng.wait_ge(dsem[k], v)
                                known_d[k] = v
                        ins = op.fn(eng)
                        if op.is_dma:
                            ins.then_inc(dsem[op.dsem], 16)
                        elif op.signal:
                            ins.then_inc(esem[eng_name], 1)
                    if eng_name == "sync":
                        for (e, s), c in self.dma_cnt.items():
                            eng.wait_ge(dsem[(e, s)], 16 * c)
                return body

            for e in ENGS:
                if self.ops[e] or e == "sync":
                    getattr(block, e)(run(e))


class Tl:
    __slots__ = ("ap", "key")

    def __init__(self, ap, key):
        self.ap = ap
        self.key = key

    def __getitem__(self, idx):
        return self.ap[idx]


_DT_BYTES = {F32: 4, BF16: 2, U32: 4}


class Arena:
    def __init__(self, nc, st, nbytes):
        self.t = st.enter_context(nc.sbuf_tensor("arena", [128, nbytes // 4], F32))
        self.words = nbytes // 4
        self.off = 0
        self.gen = 0

    def reset(self):
        self.off = 0
        self.gen += 1

    def alloc(self, name, free_shape, dt):
        n = 1
        for s in free_shape:
            n *= s
        words = (n * _DT_BYTES[dt] + 3) // 4
        words = (words + 7) // 8 * 8
        assert self.off + words <= self.words, "arena overflow at %s: %d + %d > %d" % (name, self.off * 4, words * 4, self.words * 4)
        ap = self.t[:, self.off:self.off + words]
        self.off += words
        if dt != F32:
            ap = ap.bitcast(dt)
        ap = ap[:, 0:n]
        if len(free_shape) == 2:
            ap = ap.rearrange("p (a b) -> p a b", a=free_shape[0])
        elif len(free_shape) == 3:
            ap = ap.rearrange("p (a b c) -> p a b c", a=free_shape[0], b=free_shape[1])
        elif len(free_shape) == 4:
            ap = ap.rearrange("p (a b c d) -> p a b c d", a=free_shape[0], b=free_shape[1], c=free_shape[2])
        return Tl(ap, "%s#%d" % (name, self.gen))


def build_program(D, T, nph=None, dbg=False, stub_peer=False):
    KC = D // 128
    W2 = 2 * T
    NTB = T // 512
    NWB = W2 // 512
    NTT = T // 128
    NWT = W2 // 128
    HA = D // 256
    HR = D // 512
    NAB = 1 + NTB
    NKP = NAB * 4
    XH = 4
    DX = D // XH
    CX = DX // 128
    MEM = 256
    NE = 128
    gam = [1.0 - 2.0 ** (-5.0 - h) for h in range(HR)]

    nc = bass.Bass("TRN2", target_bir_lowering=False)

    def din(name, shape, dt=F32):
        return nc.dram_tensor(name, list(shape), dt, kind="ExternalInput").ap()

    def dscr(name, shape, dt):
        if dbg:
            return nc.dram_tensor(name, list(shape), dt, kind="ExternalOutput").ap()
        return nc.dram_tensor(name, list(shape), dt).ap()

    xw = din("xw", [W2, D])
    memb = din("memb", [MEM, D])
    n1w = din("n1w", [128, D])
    n2w = din("n2w", [128, D])
    nmw = din("nmw", [128, D])
    n3w = din("n3w", [128, D])
    nfw = din("nfw", [128, D])
    w_attn = din("w_attn", [D, HA * 384])
    w_ret = din("w_ret", [D, HR * 768])
    relbT = din("relbT", [HA, 128, 640])
    kmask = din("kmask", [128, NKP])
    costab = din("costab", [128, W2])
    sintab = din("sintab", [128, W2])
    mtab = din("mtab", [128, HR * 128])
    gtab = din("gtab", [128, HR * 128])
    kdec = din("kdec", [128, HR * NWT])
    gnw = din("gnw", [128, HR * 256])
    w_out = din("w_out", [D, D])
    wq = din("wq", [D, D])
    wkv = din("wkv", [D, 2 * D])
    wo = din("wo", [D, D])
    pwq = din("pwq", [D, 2048])
    subT = din("subT", [128, 16 * 128])
    uT = din("uT", [D, NE * 128] if not stub_peer else [128, 128])
    pv = din("pv", [NE * 128, D] if not stub_peer else [128, 128])
    consts = din("consts", [128, 4 * 128])
    out = nc.dram_tensor("out", [T, D], F32, kind="ExternalOutput").ap()

    xnT_d = dscr("xnT_d", [NWB, 128, KC, 512], BF16)
    mixT_d = dscr("mixT_d", [NTB, 128, KC, 512], BF16)
    h1_d = dscr("h1_d", [T, D], F32)
    hn2T_d = dscr("hn2T_d", [NTB, 128, KC, 512], BF16)
    memT_d = dscr("memT_d", [1, 128, KC, MEM], BF16)
    oT_d = dscr("oT_d", [NTB, 128, KC, 512], BF16)
    h2_d = dscr("h2_d", [T, D], F32)
    hn3T_d = dscr("hn3T_d", [NTB, 128, KC, 512], BF16)
    G_d = dscr("G_d", [NE, 128, T], BF16)
    GA_d = dscr("GA_d", [NE, 128, T], BF16)
    h3_d = dscr("h3_d", [T, D], F32)

    P = Prog(nc)
    st = contextlib.ExitStack()
    with st:
        A = Arena(nc, st, 200 * 1024)
        pb = [Tl(st.enter_context(nc.psum_tensor("pb%d" % i, [128, 512], F32))[:], "pb%d" % i) for i in range(8)]
        cst_f = st.enter_context(nc.sbuf_tensor("cst_f", [128, 512], F32))
        cst_b = st.enter_context(nc.sbuf_tensor("cst_b", [128, 512], BF16))
        identf = cst_f[:, 0:128]
        iotaf = cst_f[:, 384:512]
        identb = cst_b[:, 0:128]
        permb = cst_b[:, 128:256]
        onesb = cst_b[:, 256:384]
        CK = ["cst"]

        def DMA(eng, o, i, reads, writes):
            P.add(eng, lambda e: e.dma_start(out=o, in_=i), reads, writes, dma=True)

        def MM(o, lhsT, rhs, start, stop, reads, writes):
            P.add("tensor", lambda e: e.matmul(o, lhsT=lhsT, rhs=rhs, start=start, stop=stop), reads, writes)

        def TR(o, i, ident, reads, writes):
            P.add("tensor", lambda e: e.transpose(out=o, in_=i, identity=ident), reads, writes)

        def ACT(o, i, func, reads, writes, bias=None, scale=None, accum=None):
            kw = {}
            if bias is not None:
                kw["bias"] = bias
            if scale is not None:
                kw["scale"] = scale
            if accum is not None:
                kw["accum_out"] = accum
            P.add("scalar", lambda e: e.activation(out=o, in_=i, func=func, **kw), reads, writes)

        def CP(eng, o, i, reads, writes):
            if eng == "scalar":
                P.add("scalar", lambda e: e.activation(out=o, in_=i, func=AF.Copy), reads, writes)
            else:
                P.add(eng, lambda e: e.tensor_copy(out=o, in_=i), reads, writes)

        def TT(o, a, b, op, reads, writes, eng="vector"):
            P.add(eng, lambda e: e.tensor_tensor(out=o, in0=a, in1=b, op=op), reads, writes)

        def TS(o, a, s1, s2, op0, op1, reads, writes, eng="vector"):
            if op1 is None:
                P.add(eng, lambda e: e.tensor_scalar(out=o, in0=a, scalar1=s1, scalar2=None, op0=op0), reads, writes)
            else:
                P.add(eng, lambda e: e.tensor_scalar(out=o, in0=a, scalar1=s1, scalar2=s2, op0=op0, op1=op1), reads, writes)

        def STT(o, a, s, b, op0, op1, reads, writes):
            P.add("vector", lambda e: e.scalar_tensor_tensor(out=o, in0=a, scalar=s, in1=b, op0=op0, op1=op1), reads, writes)

        def RECIP(o, i, reads, writes):
            P.add("vector", lambda e: e.reciprocal(out=o, in_=i), reads, writes)

        def wview(w_ap, c0, c1):
            return w_ap[:, c0:c1].rearrange("(kc p) n -> p kc n", p=128)

        def load_w(dst, w_ap, c0, c1, kcn, reads=()):
            v = wview(w_ap, c0, c1)
            step = max(1, 1024 // 128)
            for k0 in range(0, kcn, step):
                k1 = min(kcn, k0 + step)
                for n0 in range(0, c1 - c0, 512):
                    n1 = min(c1 - c0, n0 + 512)
                    DMA("gpsimd", dst.ap[:, k0:k1, n0:n1], v[:, k0:k1, n0:n1], list(reads), [dst.key])

        evac_rr = [0]

        def evac_eng():
            evac_rr[0] += 1
            return "scalar" if evac_rr[0] % 2 else "vector"

        DMA("sync", cst_f[:], consts, [], CK)
        CP("vector", cst_b[:], cst_f[:], CK, ["cstb"])
        CKB = ["cstb"]

        def phase_nt(src, ntok, wrep_d, dst_d, blk, final_out=None):
            A.reset()
            wrep = A.alloc("wrep", [D], F32)
            xt = [A.alloc("xt%d" % i, [D], F32) for i in range(2)]
            junk = A.alloc("junk", [D], BF16)
            ss = [A.alloc("ss%d" % i, [1], F32) for i in range(2)]
            rs = [A.alloc("rs%d" % i, [1], F32) for i in range(2)]
            if final_out is None:
                xb = [A.alloc("xb%d" % i, [D], BF16) for i in range(2)]
                xT = [A.alloc("xT%d" % i, [KC, 128], BF16) for i in range(2)]
            else:
                xo = [A.alloc("xo%d" % i, [D], F32) for i in range(2)]
            DMA("sync", wrep.ap, wrep_d, [], [wrep.key])
            nt = ntok // 128
            per = blk // 128
            bank = 0
            SK = [src.tensor.name]
            DMA("sync", xt[0].ap, src[0:128, :], SK, [xt[0].key])
            for i in range(nt):
                s = i % 2
                if i + 1 < nt:
                    DMA("sync", xt[(i + 1) % 2].ap, src[(i + 1) * 128:(i + 2) * 128, :], SK, [xt[(i + 1) % 2].key])
                ACT(junk.ap, xt[s].ap, AF.Square, [xt[s].key], [junk.key, ss[s].key], accum=ss[s].ap)
                TS(rs[s].ap, ss[s].ap, 1.0 / D, EPS, ALU.mult, ALU.add, [ss[s].key], [rs[s].key])
                ACT(rs[s].ap, rs[s].ap, AF.Sqrt, [rs[s].key], [rs[s].key])
                RECIP(rs[s].ap, rs[s].ap, [rs[s].key], [rs[s].key])
                if final_out is not None:
                    STT(xo[s].ap, xt[s].ap, rs[s].ap, wrep.ap, ALU.mult, ALU.mult, [xt[s].key, rs[s].key, wrep.key], [xo[s].key])
                    DMA("sync", final_out[i * 128:(i + 1) * 128, :], xo[s].ap, [xo[s].key], [])
                    continue
                STT(xb[s].ap, xt[s].ap, rs[s].ap, wrep.ap, ALU.mult, ALU.mult, [xt[s].key, rs[s].key, wrep.key], [xb[s].key])
                for g in range(KC // 8):
                    pt = pb[bank % 4]
                    bank += 1
                    ptv = pt.ap.bitcast(BF16).rearrange("p (a b) -> p a b", a=8)
                    for j in range(8):
                        c = g * 8 + j
                        TR(ptv[:, j, :], xb[s].ap[:, c * 128:(c + 1) * 128], identb, [xb[s].key] + CKB, [pt.key])
                    CP(evac_eng(), xT[s].ap[:, g * 8:(g + 1) * 8, :], ptv, [pt.key], [xT[s].key])
                b_, sub = divmod(i, per)
                DMA("sync", dst_d[b_, :, :, sub * 128:(sub + 1) * 128], xT[s].ap, [xT[s].key], [dst_d.tensor.name])

        def phase_attn():
            A.reset()
            wA = [A.alloc("wA%d" % i, [KC, 384], BF16) for i in range(2)]
            xblk = [A.alloc("xblk%d" % i, [KC, 512], BF16) for i in range(2)]
            km = A.alloc("km", [NKP], F32)
            qT = A.alloc("qT", [T], BF16)
            kT = A.alloc("kT", [NAB * 512], BF16)
            vA = A.alloc("vA", [NKP, 128], BF16)
            bT = [A.alloc("bT%d" % i, [640], BF16) for i in range(2)]
            PT = A.alloc("PT", [NKP, 640], BF16)
            oT = [A.alloc("oT%d" % i, [T], BF16) for i in range(2)]
            rc = [A.alloc("rc%d" % i, [128], F32) for i in range(2)]
            ovs = [A.alloc("ov%d" % i, [128], F32) for i in range(2)]
            DMA("sync", km.ap, kmask, [], [km.key])
            xcnt = 0
            sc = 128 ** -0.5
            XD = [xnT_d.tensor.name]
            for h in range(HA):
                w = wA[h % 2]
                load_w(w, w_attn, h * 384, (h + 1) * 384, KC)
                b = bT[h % 2]
                DMA("gpsimd", b.ap, relbT[h], [], [b.key])
                for tbi in range(NAB):
                    tb = NWB - NAB + tbi
                    xb = xblk[xcnt % 2]
                    xcnt += 1
                    DMA("sync", xb.ap, xnT_d[tb], XD, [xb.key])
                    pk = pb[0]
                    for kc in range(KC):
                        MM(pk.ap, w.ap[:, kc, 128:256], xb.ap[:, kc, :], kc == 0, kc == KC - 1, [w.key, xb.key], [pk.key])
                    CP("scalar", kT.ap[:, tbi * 512:(tbi + 1) * 512], pk.ap, [pk.key], [kT.key])
                    if tbi >= 1:
                        pq = pb[1]
                        for kc in range(KC):
                            MM(pq.ap, w.ap[:, kc, 0:128], xb.ap[:, kc, :], kc == 0, kc == KC - 1, [w.key, xb.key], [pq.key])
                        TS(qT.ap[:, (tbi - 1) * 512:tbi * 512], pq.ap, sc, None, ALU.mult, None, [pq.key], [qT.key])
                    pvv = pb[2 + (tbi % 2)]
                    for j in range(4):
                        for kc in range(KC):
                            MM(pvv.ap[:, j * 128:(j + 1) * 128], xb.ap[:, kc, j * 128:(j + 1) * 128], w.ap[:, kc, 256:384],
                               kc == 0, kc == KC - 1, [w.key, xb.key], [pvv.key])
                    CP("vector", vA.ap[:, tbi * 4:(tbi + 1) * 4, :], pvv.ap.rearrange("p (a b) -> p a b", a=4), [pvv.key], [vA.key])
                import os as _os
                _cut = int(_os.environ.get('ATTN_CUT', '9'))
                if _cut < 2:
                    continue
                for kt in range(NKP):
                    q_lo = max(kt, 4)
                    q_hi = min(kt + 4, NKP - 1)
                    nq = (q_hi - q_lo + 1) * 128
                    jq0 = q_lo - kt
                    parts = [(0, min(nq, 512))]
                    if nq > 512:
                        parts.append((512, nq))
                    for pi, (c0, c1) in enumerate(parts):
                        ps = pb[4 + ((2 * kt + pi) % 4)]
                        n = c1 - c0
                        MM(ps.ap[:, 0:n], kT.ap[:, kt * 128:(kt + 1) * 128], qT.ap[:, (q_lo - 4) * 128 + c0:(q_lo - 4) * 128 + c1],
                           True, False, [kT.key, qT.key], [ps.key])
                        MM(ps.ap[:, 0:n], identb, b.ap[:, jq0 * 128 + c0:jq0 * 128 + c1], False, True, [b.key] + CKB, [ps.key])
                        ACT(PT.ap[:, kt, c0:c1], ps.ap[:, 0:n], AF.Exp, [ps.key, km.key], [PT.key + ":%d" % kt], bias=km.ap[:, kt:kt + 1])
                if _cut < 3:
                    continue
                o = oT[h % 2]
                for qo in range(NTT):
                    qt = qo + 4
                    po = pb[(qo % 2)]
                    kts = list(range(qt - 4, qt + 1))
                    for gi, (lhs, c0) in enumerate(((None, 0), (onesb, 128))):
                        for n_, kt in enumerate(kts):
                            off = (qt - max(kt, 4)) * 128
                            l = vA.ap[:, kt, :] if lhs is None else lhs
                            MM(po.ap[:, c0:c0 + 128], l, PT.ap[:, kt, off:off + 128], n_ == 0, n_ == len(kts) - 1,
                               [vA.key, PT.key + ":%d" % kt] + CKB, [po.key])
                    r = rc[qo % 2]
                    CP("scalar", r.ap, po.ap[:, 128:256], [po.key], [r.key])
                    RECIP(r.ap, r.ap, [r.key], [r.key])
                    ov = ovs[qo % 2]
                    CP("scalar", ov.ap, po.ap[:, 0:128], [po.key], [ov.key])
                    TT(o.ap[:, qo * 128:(qo + 1) * 128], ov.ap, r.ap, ALU.mult, [ov.key, r.key], [o.key])
                for tb in range(NTB):
                    DMA("sync", mixT_d[tb, :, h, :], o.ap[:, tb * 512:(tb + 1) * 512], [o.key], [mixT_d.tensor.name])

        def phase_ret():
            A.reset()
            W = A.alloc("wR", [KC, 768], BF16)
            xblk = [A.alloc("xblk%d" % i, [KC, 512], BF16) for i in range(2)]
            cs = [A.alloc("cs%d" % i, [512], F32) for i in range(2)]
            sn = [A.alloc("sn%d" % i, [512], F32) for i in range(2)]
            mt = A.alloc("mt", [HR, 128], F32)
            gt = A.alloc("gt", [HR, 128], F32)
            kd = A.alloc("kd", [HR, NWT], F32)
            gw = A.alloc("gw", [HR, 256], F32)
            krT = A.alloc("krT", [W2], BF16)
            qrT = A.alloc("qrT", [T], BF16)
            qtT = A.alloc("qtT", [T], BF16)
            ktm = A.alloc("ktm", [NWT, 128], BF16)
            vR = A.alloc("vR", [NWT, 256], BF16)
            sg = A.alloc("sg", [NTT, 256], BF16)
            raw = [A.alloc("raw%d" % i, [512], BF16) for i in range(2)]
            raw2 = [A.alloc("raw2%d" % i, [512], BF16) for i in range(2)]
            t1 = [A.alloc("t1%d" % i, [512], F32) for i in range(2)]
            t2 = [A.alloc("t2%d" % i, [512], F32) for i in range(2)]
            S = A.alloc("S", [256], F32)
            Sb = A.alloc("Sb", [256], BF16)
            AT = [A.alloc("AT%d" % i, [128], BF16) for i in range(2)]
            st6 = A.alloc("st6", [6], F32)
            mv = A.alloc("mv", [2], F32)
            rstd = A.alloc("rstd", [1], F32)
            yn = [A.alloc("yn%d" % i, [256], F32) for i in range(2)]
            rr = [A.alloc("rr%d" % i, [256], BF16) for i in range(2)]
            rT = A.alloc("rT", [2, T], BF16)
            tis = [A.alloc("ti%d" % i, [128], F32) for i in range(2)]
            kvs = A.alloc("kvs", [256], F32)
            ysbs = [A.alloc("ysb%d" % i, [256], F32) for i in range(2)]
            import os as _os
            _sub = _os.environ.get('RET_SUB', '')
            if 'd' not in _sub:
                DMA("sync", mt.ap, mtab.rearrange("p (h c) -> p h c", h=HR), [], [mt.key])
                DMA("sync", gt.ap, gtab.rearrange("p (h c) -> p h c", h=HR), [], [gt.key])
                DMA("sync", kd.ap, kdec.rearrange("p (h c) -> p h c", h=HR), [], [kd.key])
                DMA("sync", gw.ap, gnw.rearrange("p (h c) -> p h c", h=HR), [], [gw.key])
            XD = [xnT_d.tensor.name]
            xcnt = 0
            rcnt = 0

            def rotary(ps, dst_ap, dst_key, c, s_):
                nonlocal rcnt
                i = rcnt % 2
                rcnt += 1
                if 'r' in _sub:
                    CP("scalar", dst_ap, ps.ap, [ps.key], [dst_key])
                    return
                _rn = int(_os.environ.get('ROT_N', '9'))
                CP("scalar", raw[i].ap, ps.ap, [ps.key], [raw[i].key])
                psw = pb[6 + i]
                if _rn >= 2:
                    MM(psw.ap, permb, raw[i].ap, True, True, [raw[i].key] + CKB, [psw.key])
                if _rn >= 3:
                    TT(t1[i].ap, raw[i].ap, c.ap, ALU.mult, [raw[i].key, c.key], [t1[i].key])
                if _rn >= 4:
                    CP("scalar", raw2[i].ap, psw.ap, [psw.key], [raw2[i].key])
                    TT(t2[i].ap, raw2[i].ap, s_.ap, ALU.mult, [raw2[i].key, s_.key], [t2[i].key])
                if _rn >= 5:
                    TT(dst_ap, t1[i].ap, t2[i].ap, ALU.add, [t1[i].key, t2[i].key], [dst_key])

            import os as _os
            _sub = _os.environ.get('RET_SUB', '')
            for h in range(HR if 'x' not in _sub else 0):
                load_w(W, w_ret, h * 768, (h + 1) * 768, KC)
                if 'w' in _sub:
                    continue
                for tb in range(NWB):
                    own = tb >= NWB // 2
                    ob = tb - NWB // 2
                    xb = xblk[xcnt % 2]
                    c_ = cs[xcnt % 2]
                    s_ = sn[xcnt % 2]
                    xcnt += 1
                    DMA("sync", xb.ap, xnT_d[tb], XD, [xb.key])
                    DMA("sync", c_.ap, costab[:, tb * 512:(tb + 1) * 512], [], [c_.key])
                    DMA("sync", s_.ap, sintab[:, tb * 512:(tb + 1) * 512], [], [s_.key])
                    pk = pb[0]
                    for kc in range(KC):
                        MM(pk.ap, W.ap[:, kc, 128:256], xb.ap[:, kc, :], kc == 0, kc == KC - 1, [W.key, xb.key], [pk.key])
                    rotary(pk, krT.ap[:, tb * 512:(tb + 1) * 512], krT.key + ":%d" % tb, c_, s_)
                    if own and 'q' not in _sub:
                        pq = pb[1]
                        for kc in range(KC):
                            MM(pq.ap, W.ap[:, kc, 0:128], xb.ap[:, kc, :], kc == 0, kc == KC - 1, [W.key, xb.key], [pq.key])
                        rotary(pq, qrT.ap[:, ob * 512:(ob + 1) * 512], qrT.key + ":%d" % ob, c_, s_)
                        TT(qtT.ap[:, ob * 512:(ob + 1) * 512].rearrange("p (a b) -> p a b", a=4),
                           qrT.ap[:, ob * 512:(ob + 1) * 512].rearrange("p (a b) -> p a b", a=4),
                           gt.ap[:, h:h + 1, :].broadcast_to([128, 4, 128]), ALU.mult,
                           [qrT.key + ":%d" % ob, gt.key], [qtT.key + ":%d" % ob])
                    for j in range(4 if 'v' not in _sub else 0):
                        wt = tb * 4 + j
                        pvv = pb[2 + (j % 2)]
                        for kc in range(KC):
                            MM(pvv.ap[:, 0:256], xb.ap[:, kc, j * 128:(j + 1) * 128], W.ap[:, kc, 256:512], kc == 0, kc == KC - 1,
                               [W.key, xb.key], [pvv.key])
                        CP(evac_eng(), vR.ap[:, wt, :], pvv.ap[:, 0:256], [pvv.key], [vR.key + ":%d" % wt])
                        if own:
                            pg = pb[4 + (j % 2)]
                            for kc in range(KC):
                                MM(pg.ap[:, 0:256], xb.ap[:, kc, j * 128:(j + 1) * 128], W.ap[:, kc, 512:768], kc == 0, kc == KC - 1,
                                   [W.key, xb.key], [pg.key])
                            ACT(sg.ap[:, ob * 4 + j, :], pg.ap[:, 0:256], AF.Silu, [pg.key], [sg.key + ":%d" % (ob * 4 + j)])
                    ptk = pb[4 + (tb % 2)] if not own else pb[6 + (tb % 2)]
                    ptkv = ptk.ap.bitcast(BF16).rearrange("p (a b) -> p a b", a=8)
                    for j in range(4 if 't' not in _sub else 0):
                        wt = tb * 4 + j
                        TR(ptkv[:, j, :], krT.ap[:, wt * 128:(wt + 1) * 128], identb, [krT.key + ":%d" % tb] + CKB, [ptk.key])
                        TS(ktm.ap[:, wt, :], ptkv[:, j, :], kd.ap[:, h, wt:wt + 1], None, ALU.mult, None, [ptk.key, kd.key],
                           [ktm.key + ":%d" % wt])
                import os as _os
                _rc = int(_os.environ.get('RET_CUT', '9'))
                if _rc < 2:
                    continue
                pS = pb[0]
                npre = NWT // 2
                for wt in range(npre):
                    MM(pS.ap[:, 0:256], ktm.ap[:, wt, :], vR.ap[:, wt, :], wt == 0, wt == npre - 1,
                       [ktm.key + ":%d" % wt, vR.key + ":%d" % wt], [pS.key])
                CP("scalar", S.ap, pS.ap[:, 0:256], [pS.key], [S.key])
                CP("vector", Sb.ap, S.ap, [S.key], [Sb.key])
                g128 = gam[h] ** 128
                if _rc < 3:
                    continue
                for b_ in range(NTT):
                    wt = npre + b_
                    ob = b_ // 4
                    pi = pb[1 + (b_ % 2)]
                    MM(pi.ap[:, 0:128], krT.ap[:, wt * 128:(wt + 1) * 128], qrT.ap[:, b_ * 128:(b_ + 1) * 128], True, True,
                       [krT.key + ":%d" % (wt // 4), qrT.key + ":%d" % ob], [pi.key])
                    at = AT[b_ % 2]
                    ti = tis[b_ % 2]
                    CP("scalar", ti.ap, pi.ap[:, 0:128], [pi.key], [ti.key])
                    TT(at.ap, ti.ap, mt.ap[:, h, :], ALU.mult, [ti.key, mt.key], [at.key])
                    py = pb[3 + (b_ % 2)]
                    MM(py.ap[:, 0:256], at.ap, vR.ap[:, wt, :], True, False, [at.key, vR.key + ":%d" % wt], [py.key])
                    MM(py.ap[:, 0:256], qtT.ap[:, b_ * 128:(b_ + 1) * 128], Sb.ap, False, True, [qtT.key + ":%d" % ob, Sb.key], [py.key])
                    if b_ < NTT - 1:
                        pkv = pb[5]
                        MM(pkv.ap[:, 0:256], ktm.ap[:, wt, :], vR.ap[:, wt, :], True, True,
                           [ktm.key + ":%d" % wt, vR.key + ":%d" % wt], [pkv.key])
                        CP("scalar", kvs.ap, pkv.ap[:, 0:256], [pkv.key], [kvs.key])
                        STT(S.ap, S.ap, g128, kvs.ap, ALU.mult, ALU.add, [S.key, kvs.key], [S.key])
                        CP("scalar", Sb.ap, S.ap, [S.key], [Sb.key])
                    if _rc < 4:
                        continue
                    ysb = ysbs[b_ % 2]
                    CP("scalar", ysb.ap, py.ap[:, 0:256], [py.key], [ysb.key])
                    P.add("vector", lambda e, o_=st6.ap, i_=ysb.ap: e.bn_stats(out=o_, in_=i_), [ysb.key], [st6.key])
                    P.add("vector", lambda e, o_=mv.ap, i_=st6.ap: e.bn_aggr(out=o_, in_=i_), [st6.key], [mv.key])
                    TS(rstd.ap, mv.ap[:, 1:2], EPS, None, ALU.add, None, [mv.key], [rstd.key])
                    ACT(rstd.ap, rstd.ap, AF.Sqrt, [rstd.key], [rstd.key])
                    RECIP(rstd.ap, rstd.ap, [rstd.key], [rstd.key])
                    y_ = yn[b_ % 2]
                    TS(y_.ap, ysb.ap, mv.ap[:, 0:1], rstd.ap, ALU.subtract, ALU.mult, [ysb.key, mv.key, rstd.key], [y_.key])
                    TT(y_.ap, y_.ap, gw.ap[:, h, :], ALU.mult, [y_.key, gw.key], [y_.key])
                    r_ = rr[b_ % 2]
                    TT(r_.ap, y_.ap, sg.ap[:, b_, :], ALU.mult, [y_.key, sg.key + ":%d" % b_], [r_.key])
                    ptr = pb[6 + (b_ % 2)]
                    ptrv = ptr.ap.bitcast(BF16).rearrange("p (a b) -> p a b", a=8)
                    for jj in range(2):
                        TR(ptrv[:, jj, :], r_.ap[:, jj * 128:(jj + 1) * 128], identb, [r_.key] + CKB, [ptr.key])
                    CP("scalar", rT.ap[:, :, b_ * 128:(b_ + 1) * 128], ptrv[:, 0:2, :], [ptr.key], [rT.key])
                for jj in range(2):
                    for tb in range(NTB):
                        DMA("sync", mixT_d[tb, :, HA + 2 * h + jj, :], rT.ap[:, jj, tb * 512:(tb + 1) * 512], [rT.key], [mixT_d.tensor.name])

        def phase_linear_A(actT_d, w_ap, Dout, res_d, dst_d):
            A.reset()
            act = [A.alloc("act%d" % i, [KC, 512], BF16) for i in range(NTB)]
            wb = [A.alloc("wb%d" % i, [KC, 512], BF16) for i in range(2)]
            rt = [A.alloc("rt%d" % i, [512], F32) for i in range(3)]
            ot = [A.alloc("ot%d" % i, [512], F32) for i in range(3)]
            for tb in range(NTB):
                DMA("sync", act[tb].ap, actT_d[tb], [actT_d.tensor.name], [act[tb].key])
            ncb = Dout // 512
            load_w(wb[0], w_ap, 0, 512, KC)
            cnt = 0
            for cb in range(ncb):
                w = wb[cb % 2]
                if cb + 1 < ncb:
                    load_w(wb[(cb + 1) % 2], w_ap, (cb + 1) * 512, (cb + 2) * 512, KC)
                for tt in range(NTT):
                    s = cnt % 3
                    pbk = pb[cnt % 4]
                    cnt += 1
                    DMA("sync", rt[s].ap, res_d[tt * 128:(tt + 1) * 128, cb * 512:(cb + 1) * 512], [res_d.tensor.name], [rt[s].key])
                    a = act[tt // 4]
                    for kc in range(KC):
                        MM(pbk.ap, a.ap[:, kc, (tt % 4) * 128:(tt % 4 + 1) * 128], w.ap[:, kc, :], kc == 0, kc == KC - 1,
                           [a.key, w.key], [pbk.key])
                    CP("scalar", ot[s].ap, pbk.ap, [pbk.key], [ot[s].key])
                    TT(ot[s].ap, ot[s].ap, rt[s].ap, ALU.add, [ot[s].key, rt[s].key], [ot[s].key])
                    DMA("sync", dst_d[tt * 128:(tt + 1) * 128, cb * 512:(cb + 1) * 512], ot[s].ap, [ot[s].key], [dst_d.tensor.name])

        def phase_xattn():
            A.reset()
            hn = [A.alloc("hn%d" % i, [KC, 512], BF16) for i in range(NTB)]
            wb = [A.alloc("wb%d" % i, [KC, 512], BF16) for i in range(2)]
            kTx = A.alloc("kTx", [KC, MEM], BF16)
            vX = A.alloc("vX", [2, D], BF16)
            mark = A.off
            mT = A.alloc("mT", [KC, MEM], BF16)
            DMA("sync", mT.ap, memT_d[0], [memT_d.tensor.name], [mT.key])
            for tb in range(NTB):
                DMA("sync", hn[tb].ap, hn2T_d[tb], [hn2T_d.tensor.name], [hn[tb].key])
            wcnt = 0
            bcnt = 0
            for cb in range(D // 512):
                w = wb[wcnt % 2]
                wcnt += 1
                load_w(w, wkv, cb * 512, (cb + 1) * 512, KC)
                for c4 in range(4):
                    pk = pb[bcnt % 4]
                    bcnt += 1
                    for kc in range(KC):
                        MM(pk.ap[:, 0:MEM], w.ap[:, kc, c4 * 128:(c4 + 1) * 128], mT.ap[:, kc, :], kc == 0, kc == KC - 1,
                           [w.key, mT.key], [pk.key])
                    CP(evac_eng(), kTx.ap[:, cb * 4 + c4, :], pk.ap[:, 0:MEM], [pk.key], [kTx.key])
            for cb in range(D // 512):
                w = wb[wcnt % 2]
                wcnt += 1
                load_w(w, wkv, D + cb * 512, D + (cb + 1) * 512, KC)
                for mtile in range(2):
                    pvv = pb[bcnt % 4]
                    bcnt += 1
                    for kc in range(KC):
                        MM(pvv.ap, mT.ap[:, kc, mtile * 128:(mtile + 1) * 128], w.ap[:, kc, :], kc == 0, kc == KC - 1,
                           [w.key, mT.key], [pvv.key])
                    CP(evac_eng(), vX.ap[:, mtile, cb * 512:(cb + 1) * 512], pvv.ap, [pvv.key], [vX.key])
            P.barrier()
            A.off = mark
            A.gen += 1
            qTx = A.alloc("qTx", [CX, T], BF16)
            PTx = [A.alloc("PTx%d" % i, [512], BF16) for i in range(2)]
            rcx = A.alloc("rcx", [512], F32)
            oTx = [A.alloc("oTx%d" % i, [512], BF16) for i in range(3)]
            otmp = A.alloc("otmp", [512], F32)
            sc = DX ** -0.5
            ocnt = 0
            for hx in range(XH):
                for cb2 in range(DX // 512):
                    w = wb[wcnt % 2]
                    wcnt += 1
                    c0 = hx * DX + cb2 * 512
                    load_w(w, wq, c0, c0 + 512, KC)
                    for tb in range(NTB):
                        for c4 in range(4):
                            pq = pb[bcnt % 4]
                            bcnt += 1
                            for kc in range(KC):
                                MM(pq.ap, w.ap[:, kc, c4 * 128:(c4 + 1) * 128], hn[tb].ap[:, kc, :], kc == 0, kc == KC - 1,
                                   [w.key, hn[tb].key], [pq.key])
                            TS(qTx.ap[:, cb2 * 4 + c4, tb * 512:(tb + 1) * 512], pq.ap, sc, None, ALU.mult, None, [pq.key], [qTx.key])
                for tb in range(NTB):
                    for kt in range(2):
                        ps = pb[4 + kt]
                        for c in range(CX):
                            MM(ps.ap, kTx.ap[:, hx * CX + c, kt * 128:(kt + 1) * 128], qTx.ap[:, c, tb * 512:(tb + 1) * 512],
                               c == 0, c == CX - 1, [kTx.key, qTx.key], [ps.key])
                        ACT(PTx[kt].ap, ps.ap, AF.Exp, [ps.key], [PTx[kt].key])
                    pd = pb[6]
                    for kt in range(2):
                        MM(pd.ap, onesb, PTx[kt].ap, kt == 0, kt == 1, [PTx[kt].key] + CKB, [pd.key])
                    CP("scalar", rcx.ap, pd.ap, [pd.key], [rcx.key])
                    RECIP(rcx.ap, rcx.ap, [rcx.key], [rcx.key])
                    for c in range(CX):
                        po = pb[bcnt % 4]
                        bcnt += 1
                        fc = hx * CX + c
                        for kt in range(2):
                            MM(po.ap, vX.ap[:, kt, fc * 128:(fc + 1) * 128], PTx[kt].ap, kt == 0, kt == 1,
                               [vX.key, PTx[kt].key], [po.key])
                        o = oTx[ocnt % 3]
                        ocnt += 1
                        CP("scalar", otmp.ap, po.ap, [po.key], [otmp.key])
                        TT(o.ap, otmp.ap, rcx.ap, ALU.mult, [otmp.key, rcx.key], [o.key])
                        DMA("sync", oT_d[tb, :, fc, :], o.ap, [o.key], [oT_d.tensor.name])

        def phase_peer_qr():
            A.reset()
            qpT = A.alloc("qpT", [16, T], BF16)
            sub = A.alloc("sub", [16, 128], BF16)
            mark = A.off
            hn = [A.alloc("hn%d" % i, [KC, 512], BF16) for i in range(NTB)]
            wb = [A.alloc("wb%d" % i, [KC, 512], BF16) for i in range(2)]
            DMA("gpsimd", sub.ap, subT.rearrange("p (a b) -> p a b", a=16), [], [sub.key])
            for tb in range(NTB):
                DMA("sync", hn[tb].ap, hn3T_d[tb], [hn3T_d.tensor.name], [hn[tb].key])
            bcnt = 0
            for cb in range(4):
                w = wb[cb % 2]
                load_w(w, pwq, cb * 512, (cb + 1) * 512, KC)
                for tb in range(NTB):
                    for c4 in range(4):
                        pq = pb[bcnt % 4]
                        bcnt += 1
                        for kc in range(KC):
                            MM(pq.ap, w.ap[:, kc, c4 * 128:(c4 + 1) * 128], hn[tb].ap[:, kc, :], kc == 0, kc == KC - 1,
                               [w.key, hn[tb].key], [pq.key])
                        CP(evac_eng(), qpT.ap[:, cb * 4 + c4, tb * 512:(tb + 1) * 512], pq.ap, [pq.key], [qpT.key])
            P.barrier()
            A.off = mark
            A.gen += 1
            qk = [qpT.key]
            s = A.alloc("s", [16, 128], F32)
            s2 = A.alloc("s2", [16, 128], F32)
            T16 = A.alloc("T16", [16, 16], F32)
            I16 = A.alloc("I16", [16, 16], U32)
            I16f = A.alloc("I16f", [16, 16], F32)
            cand = A.alloc("cand", [8, 256], F32)
            cand2 = A.alloc("cand2", [8, 256], F32)
            B16 = A.alloc("B16", [8, 16], F32)
            C16 = A.alloc("C16", [8, 16], U32)
            Ai = A.alloc("Ai", [8, 16], U32)
            Bi = A.alloc("Bi", [8, 16], U32)
            Af = A.alloc("Af", [8, 16], F32)
            Bf = A.alloc("Bf", [8, 16], F32)
            eb = A.alloc("eb", [8, 16], F32)
            Z = A.alloc("Z", [8], F32)
            eq = A.alloc("eq", [128, 16], F32)
            R = A.alloc("R", [3, 128], F32)
            RT = A.alloc("RT", [3, 128], F32)
            RTb = A.alloc("RTb", [3, 128], BF16)
            OI = A.alloc("OI", [128, 128], BF16)
            OJ = A.alloc("OJ", [128, 128], BF16)
            Gt = A.alloc("Gt", [128, 128], BF16)
            subv = sub.ap
            K = lambda *a: list(a)
            for tt in range(NTT):
                for hp in range(16):
                    bk = pb[hp // 4]
                    MM(bk.ap[:, (hp % 4) * 128:(hp % 4 + 1) * 128], qpT.ap[:, hp, tt * 128:(tt + 1) * 128], subv[:, hp, :], True, True,
                       qk + [sub.key], [bk.key])
                for q in range(4):
                    CP(evac_eng(), s.ap[:, q * 4:(q + 1) * 4, :], pb[q].ap.rearrange("p (a b) -> p a b", a=4), [pb[q].key], [s.key])
                for hp in range(16):
                    sv = s.ap[:, hp, :]
                    s2v = s2.ap[:, hp, :]
                    P.add("vector", lambda e, o=T16.ap[:, hp, 0:8], i=sv: e.max(out=o, in_=i), [s.key], [T16.key])
                    P.add("vector", lambda e, o=I16.ap[:, hp, 0:8], m=T16.ap[:, hp, 0:8], i=sv: e.max_index(out=o, in_max=m, in_values=i),
                          [s.key, T16.key], [I16.key])
                    P.add("vector", lambda e, o=s2v, m=T16.ap[:, hp, 0:8], i=sv: e.match_replace(out=o, in_to_replace=m, in_values=i, imm_value=NEG),
                          [s.key, T16.key], [s2.key])
                    P.add("vector", lambda e, o=T16.ap[:, hp, 8:16], i=s2v: e.max(out=o, in_=i), [s2.key], [T16.key])
                    P.add("vector", lambda e, o=I16.ap[:, hp, 8:16], m=T16.ap[:, hp, 8:16], i=s2v: e.max_index(out=o, in_max=m, in_values=i),
                          [s2.key, T16.key], [I16.key])
                CP("vector", I16f.ap, I16.ap, [I16.key], [I16f.key])
                T16v = T16.ap.rearrange("p (h two) k -> p h two k", two=2)
                I16v = I16f.ap.rearrange("p (h two) k -> p h two k", two=2)
                candv = cand.ap.rearrange("p h (a b) -> p h a b", a=16)
                TT(candv, T16v[:, :, 0, :].unsqueeze(3).broadcast_to([128, 8, 16, 16]),
                   T16v[:, :, 1, :].unsqueeze(2).broadcast_to([128, 8, 16, 16]), ALU.add, [T16.key], [cand.key])
                for h in range(8):
                    cv = cand.ap[:, h, :]
                    c2v = cand2.ap[:, h, :]
                    P.add("vector", lambda e, o=B16.ap[:, h, 0:8], i=cv: e.max(out=o, in_=i), [cand.key], [B16.key])
                    P.add("vector", lambda e, o=C16.ap[:, h, 0:8], m=B16.ap[:, h, 0:8], i=cv: e.max_index(out=o, in_max=m, in_values=i),
                          [cand.key, B16.key], [C16.key])
                    P.add("vector", lambda e, o=c2v, m=B16.ap[:, h, 0:8], i=cv: e.match_replace(out=o, in_to_replace=m, in_values=i, imm_value=NEG),
                          [cand.key, B16.key], [cand2.key])
                    P.add("vector", lambda e, o=B16.ap[:, h, 8:16], i=c2v: e.max(out=o, in_=i), [cand2.key], [B16.key])
                    P.add("vector", lambda e, o=C16.ap[:, h, 8:16], m=B16.ap[:, h, 8:16], i=c2v: e.max_index(out=o, in_max=m, in_values=i),
                          [cand2.key, B16.key], [C16.key])
                TT(eb.ap, B16.ap, B16.ap[:, :, 0:1].broadcast_to([128, 8, 16]), ALU.subtract, [B16.key], [eb.key])
                ACT(eb.ap, eb.ap, AF.Exp, [eb.key], [eb.key])
                P.add("vector", lambda e, o=Z.ap, i=eb.ap: e.tensor_reduce(out=o, in_=i, axis=AX.X, op=ALU.add), [eb.key], [Z.key])
                RECIP(Z.ap, Z.ap, [Z.key], [Z.key])
                TT(R.ap[:, 2, :].rearrange("p (h k) -> p h k", h=8), eb.ap, Z.ap.unsqueeze(2).broadcast_to([128, 8, 16]), ALU.mult,
                   [eb.key, Z.key], [R.key + ":2"])
                P.add("vector", lambda e, o=Ai.ap, i=C16.ap: e.tensor_single_scalar(out=o, in_=i, scalar=4, op=ALU.logical_shift_right),
                      [C16.key], [Ai.key])
                P.add("vector", lambda e, o=Bi.ap, i=C16.ap: e.tensor_single_scalar(out=o, in_=i, scalar=15, op=ALU.bitwise_and),
                      [C16.key], [Bi.key])
                CP("vector", Af.ap, Ai.ap, [Ai.key], [Af.key])
                CP("vector", Bf.ap, Bi.ap, [Bi.key], [Bf.key])
                for r_i, (sel, half) in enumerate(((Af, 0), (Bf, 1))):
                    TT(eq.ap, iotaf[:, 0:16].unsqueeze(1).broadcast_to([128, 128, 16]),
                       sel.ap.rearrange("p h k -> p (h k)").unsqueeze(2).broadcast_to([128, 128, 16]), ALU.is_equal,
                       [sel.key] + CK, [eq.key])
                    eq4 = eq.ap.rearrange("p (h k) a -> p h k a", h=8)
                    TT(eq4, eq4, I16v[:, :, half, :].unsqueeze(2).broadcast_to([128, 8, 16, 16]), ALU.mult, [eq.key, I16f.key], [eq.key])
                    P.add("vector", lambda e, o=R.ap[:, r_i, :], i=eq.ap: e.tensor_reduce(out=o, in_=i, axis=AX.X, op=ALU.add),
                          [eq.key], [R.key + ":%d" % r_i])
                ptR = pb[4]
                for r_i in range(3):
                    TR(ptR.ap[:, r_i * 128:(r_i + 1) * 128], R.ap[:, r_i, :], identf, [R.key + ":%d" % r_i] + CK, [ptR.key])
                CP("scalar", RT.ap, ptR.ap[:, 0:384].rearrange("p (a b) -> p a b", a=3), [ptR.key], [RT.key])
                CP("vector", RTb.ap, RT.ap, [RT.key], [RTb.key])
                iob = cst_b[:, 384:512]
                TT(OI.ap, iob.unsqueeze(1).broadcast_to([128, 128, 128]), RTb.ap[:, 0, :].unsqueeze(2).broadcast_to([128, 128, 128]),
                   ALU.is_equal, [RTb.key] + CKB, [OI.key])
                TT(OJ.ap, iob.unsqueeze(1).broadcast_to([128, 128, 128]), RTb.ap[:, 1, :].unsqueeze(2).broadcast_to([128, 128, 128]),
                   ALU.is_equal, [RTb.key] + CKB, [OJ.key])
                TT(OJ.ap, OJ.ap, RTb.ap[:, 2, :].unsqueeze(2).broadcast_to([128, 128, 128]), ALU.mult, [OJ.key, RTb.key], [OJ.key])
                for t4 in range(32):
                    pg = pb[5 + (t4 % 3)]
                    for u in range(4):
                        t = t4 * 4 + u
                        MM(pg.ap[:, u * 128:(u + 1) * 128], OJ.ap[:, t, :], OI.ap[:, t, :], True, True, [OJ.key, OI.key], [pg.key])
                    CP(evac_eng(), Gt.ap[:, :, t4 * 4:(t4 + 1) * 4], pg.ap.rearrange("p (t i) -> p i t", t=4), [pg.key], [Gt.key])
                for i0 in range(0, 128, 16):
                    DMA("sync", G_d[i0:i0 + 16, :, tt * 128:(tt + 1) * 128].rearrange("i j t -> j i t"), Gt.ap[:, i0:i0 + 16, :],
                        [Gt.key], [G_d.tensor.name])

        def phase_peer_u():
            A.reset()
            hn = [A.alloc("hn%d" % i, [KC, 512], BF16) for i in range(NTB)]
            wb = [A.alloc("wb%d" % i, [KC, 512], BF16) for i in range(2)]
            Gi = [A.alloc("Gi%d" % i, [T], BF16) for i in range(3)]
            Aa = [A.alloc("Aa%d" % i, [512], BF16) for i in range(2)]
            GA = [A.alloc("GA%d" % i, [T], BF16) for i in range(3)]
            for tb in range(NTB):
                DMA("sync", hn[tb].ap, hn3T_d[tb], [hn3T_d.tensor.name], [hn[tb].key])
            load_w(wb[0], uT, 0, 512, KC)
            bcnt = 0
            for ib4 in range(NE // 4):
                w = wb[ib4 % 2]
                if ib4 + 1 < NE // 4:
                    load_w(wb[(ib4 + 1) % 2], uT, (ib4 + 1) * 512, (ib4 + 2) * 512, KC)
                for e4 in range(4):
                    i = ib4 * 4 + e4
                    g = Gi[i % 3]
                    ga = GA[i % 3]
                    DMA("sync", g.ap, G_d[i], [G_d.tensor.name], [g.key])
                    for tb in range(NTB):
                        pa = pb[bcnt % 6]
                        a_ = Aa[bcnt % 2]
                        bcnt += 1
                        for kc in range(KC):
                            MM(pa.ap, w.ap[:, kc, e4 * 128:(e4 + 1) * 128], hn[tb].ap[:, kc, :], kc == 0, kc == KC - 1,
                               [w.key, hn[tb].key], [pa.key])
                        ACT(a_.ap, pa.ap, AF.Gelu, [pa.key], [a_.key])
                        TT(ga.ap[:, tb * 512:(tb + 1) * 512], a_.ap, g.ap[:, tb * 512:(tb + 1) * 512], ALU.mult, [a_.key, g.key], [ga.key])
                    DMA("sync", GA_d[i], ga.ap, [ga.key], [GA_d.tensor.name])

        def phase_peer_v():
            A.reset()
            EG = 8
            vb = [A.alloc("vb%d" % i, [EG, 512], BF16) for i in range(2)]
            gg = [A.alloc("gg%d" % i, [EG, T], BF16) for i in range(2)]
            rt = [A.alloc("rt%d" % i, [512], F32) for i in range(2)]
            ot = [A.alloc("ot%d" % i, [512], F32) for i in range(2)]
            ng = NE // EG
            cnt = 0
            for cb in range(D // 512):
                for ig in range(ng):
                    v_ = vb[cnt % 2]
                    g_ = gg[cnt % 2]
                    cnt += 1
                    DMA("gpsimd", v_.ap, pv[ig * EG * 128:(ig + 1) * EG * 128, cb * 512:(cb + 1) * 512].rearrange("(e p) n -> p e n", p=128),
                        [], [v_.key])
                    DMA("sync", g_.ap, GA_d[ig * EG:(ig + 1) * EG].rearrange("e p t -> p e t"), [GA_d.tensor.name], [g_.key])
                    for e_ in range(EG):
                        first = (ig == 0 and e_ == 0)
                        last = (ig == ng - 1 and e_ == EG - 1)
                        for tt in range(NTT):
                            MM(pb[tt].ap, g_.ap[:, e_, tt * 128:(tt + 1) * 128], v_.ap[:, e_, :], first, last, [g_.key, v_.key], [pb[tt].key])
                for tt in range(NTT):
                    s = tt % 2
                    DMA("sync", rt[s].ap, h2_d[tt * 128:(tt + 1) * 128, cb * 512:(cb + 1) * 512], [h2_d.tensor.name], [rt[s].key])
                    CP("scalar", ot[s].ap, pb[tt].ap, [pb[tt].key], [ot[s].key])
                    TT(ot[s].ap, ot[s].ap, rt[s].ap, ALU.add, [ot[s].key, rt[s].key], [ot[s].key])
                    DMA("sync", h3_d[tt * 128:(tt + 1) * 128, cb * 512:(cb + 1) * 512], ot[s].ap, [ot[s].key], [h3_d.tensor.name])

        sched = [
            lambda: phase_nt(xw, W2, n1w, xnT_d, 512),
            phase_attn,
            phase_ret,
            lambda: phase_linear_A(mixT_d, w_out, D, xw[T:W2, :], h1_d),
            lambda: phase_nt(h1_d, T, n2w, hn2T_d, 512),
            lambda: phase_nt(memb, MEM, nmw, memT_d, MEM),
            phase_xattn,
            lambda: phase_linear_A(oT_d, wo, D, h1_d, h2_d),
            lambda: phase_nt(h2_d, T, n3w, hn3T_d, 512),
            phase_peer_qr,
            phase_peer_u,
            phase_peer_v,
            lambda: phase_nt(h3_d, T, nfw, None, 512, final_out=out),
        ]
        for i, ph in enumerate(sched):
            if nph is not None and i >= nph:
                break
            if i:
                P.barrier()
            ph()
        P.emit()
    return nc


def _host_tables(D, T, half):
    W2 = 2 * T
    NWT = W2 // 128
    HR = D // 512
    pos = np.arange(-T, T, dtype=np.float64) + half * T
    inv_freq = 1.0 / (10000.0 ** (np.arange(0, 128, 2, dtype=np.float64) / 128.0))
    ang = pos[None, :] * inv_freq[:, None]
    cos = np.cos(ang)
    sin = np.sin(ang)
    costab = np.concatenate([cos, cos], 0).astype(np.float32)
    sintab = np.concatenate([-sin, sin], 0).astype(np.float32)
    lg = np.log1p(-np.power(2.0, -5.0 - np.arange(HR, dtype=np.float64)))
    p = np.arange(128, dtype=np.float64)
    mk = p[:, None]
    cq = p[None, :]
    valid = (mk // 64) <= (cq // 64)
    mtab = np.zeros((128, HR, 128), np.float64)
    gtab = np.zeros((128, HR, 128), np.float64)
    kdec = np.zeros((128, HR, NWT), np.float64)
    for h in range(HR):
        mtab[:, h, :] = np.where(valid, np.exp(lg[h] * np.abs(cq - mk)), 0.0) * (128 ** -0.5)
        gtab[:, h, :] = np.exp(lg[h] * (cq + 1.0))
        for wt in range(NWT):
            if wt < NWT // 2:
                t = wt * 128 + p
                kdec[:, h, wt] = np.exp(lg[h] * (T - 1 - t)) * (128 ** -0.5)
            else:
                kdec[:, h, wt] = np.exp(lg[h] * (127 - p)) * (128 ** -0.5)
    return (costab, sintab, mtab.reshape(128, -1).astype(np.float32), gtab.reshape(128, -1).astype(np.float32),
            kdec.reshape(128, -1).astype(np.float32))


def _rel_bias_table(rb):
    mk = np.arange(128)[:, None]
    col = np.arange(640)[None, :]
    jq = col // 128
    cq = col % 128
    dist = 128 * jq + cq - mk
    idx = np.clip(dist, -256, 256) + 256
    diff = 2 * jq + (cq // 64) - (mk // 64)
    valid = (diff >= 0) & (diff <= 8)
    tab = rb[:, idx]
    return np.where(valid[None], tab, np.float32(NEG)).astype(np.float32)


_CACHE = {}


def _get_prog(D, T):
    key = (D, T)
    if key not in _CACHE:
        _CACHE[key] = build_program(D, T)
    return _CACHE[key]


def kernel(x, mem, norm1_w, w_in, attn_rel_bias, ret_gn_w, w_out, norm2_w, mem_norm_w,
           xattn_wq, xattn_wkv, xattn_wo, norm3_w, peer_wq, peer_subkeys, peer_u, peer_v,
           final_norm_w):
    f = lambda a: np.ascontiguousarray(np.asarray(a, dtype=np.float32))
    x = f(x)
    mem = f(mem)
    B, S, D = x.shape
    T = S // 2
    HA = D // 256
    HR = D // 512
    AW = HA * 128
    win = f(w_in)[0]
    aq, ak, av = win[:, 0:AW], win[:, AW:2 * AW], win[:, 2 * AW:3 * AW]
    o = 3 * AW
    rq, rk = win[:, o:o + HR * 128], win[:, o + HR * 128:o + 2 * HR * 128]
    o2 = o + 2 * HR * 128
    rv, rg = win[:, o2:o2 + HR * 256], win[:, o2 + HR * 256:o2 + 2 * HR * 256]
    w_attn = np.concatenate([np.concatenate([aq[:, h * 128:(h + 1) * 128], ak[:, h * 128:(h + 1) * 128], av[:, h * 128:(h + 1) * 128]], 1)
                             for h in range(HA)], 1)
    w_ret = np.concatenate([np.concatenate([rq[:, h * 128:(h + 1) * 128], rk[:, h * 128:(h + 1) * 128],
                                            rv[:, h * 256:(h + 1) * 256], rg[:, h * 256:(h + 1) * 256]], 1) for h in range(HR)], 1)
    rep = lambda v: np.ascontiguousarray(np.broadcast_to(f(v).reshape(1, -1), (128, f(v).size)))
    relbT = _rel_bias_table(f(attn_rel_bias)[0])
    sub = f(peer_subkeys)[0]
    subT = np.ascontiguousarray(sub.transpose(3, 0, 1, 2).reshape(128, 16 * 128))
    uT = np.ascontiguousarray(f(peer_u)[0].T)
    consts = np.zeros((128, 512), np.float32)
    consts[:, 0:128] = np.eye(128, dtype=np.float32)
    consts[:, 128:256] = np.roll(np.eye(128, dtype=np.float32), 64, axis=0)
    consts[:, 256:384] = 1.0
    consts[:, 384:512] = np.arange(128, dtype=np.float32)[None, :]
    shared = dict(
        n1w=rep(norm1_w), n2w=rep(norm2_w), nmw=rep(mem_norm_w), n3w=rep(norm3_w), nfw=rep(final_norm_w),
        w_attn=np.ascontiguousarray(w_attn), w_ret=np.ascontiguousarray(w_ret), relbT=relbT,
        gnw=rep(ret_gn_w), w_out=f(w_out)[0], wq=f(xattn_wq)[0], wkv=f(xattn_wkv)[0], wo=f(xattn_wo)[0],
        pwq=f(peer_wq)[0], subT=subT, uT=uT, pv=f(peer_v)[0], consts=consts,
    )
    NKP = (1 + T // 512) * 4
    in_maps = []
    for b in range(B):
        for half in range(2):
            xw = np.zeros((2 * T, D), np.float32)
            if half == 1:
                xw[:T] = x[b, :T]
            xw[T:] = x[b, half * T:(half + 1) * T]
            km = np.zeros((128, NKP), np.float32)
            if half == 0:
                km[:, 0:4] = NEG
            costab, sintab, mtab, gtab, kdec = _host_tables(D, T, half)
            m = dict(shared)
            m.update(xw=xw, memb=mem[b], kmask=km, costab=costab, sintab=sintab, mtab=mtab, gtab=gtab, kdec=kdec)
            in_maps.append(m)
    nc = _get_prog(D, T)
    res = run_bass_kernel_spmd(nc, in_maps, core_ids=list(range(len(in_maps))))
    outp = np.zeros((B, S, D), np.float32)
    for b in range(B):
        for half in range(2):
            outp[b, half * T:(half + 1) * T] = res.results[b * 2 + half]["out"]
    return outp
```
